# Optimizing a Trainium2 kernel written in Bass

```python
import math
import jax
import jax.numpy as jnp
from jax import lax
import numpy as np

D_MODEL = 2048
BATCH = 2
SEQ = 8192
DEPTH = 4

DN_HEADS = 6
DN_DK = 128
DN_DV = 128
DN_CHUNK = 64
CONV_W = 5
ATT_HEADS = 6
ATT_KV_HEADS = 2
ATT_DH = 128
ROPE_SUB = 64
ROPE_THETA = 10000.0
Q_BLOCK = 128
GRID_W = 64
GLA_HEADS = 4
GLA_DK = 64
GLA_DV = 128
GLA_RANK = 16
GLA_NORMALIZER = 16.0
GLA_CHUNK = 64
DN_QK = DN_HEADS * DN_DK
DN_W = DN_HEADS * DN_DV
ATT_W = ATT_HEADS * ATT_DH
ATT_KV_W = ATT_KV_HEADS * ATT_DH
GLA_QK = GLA_HEADS * GLA_DK
GLA_W = GLA_HEADS * GLA_DV
MIX_W = DN_W + ATT_W + GLA_W
IN_SIZES = (DN_QK, DN_QK, DN_W, DN_W, DN_HEADS, DN_HEADS, DN_HEADS, DN_HEADS,
            ATT_W, ATT_KV_W, ATT_KV_W,
            GLA_QK, GLA_QK, GLA_W, GLA_W, GLA_RANK, GLA_RANK)
IN_COLS = sum(IN_SIZES)
DN_CONV_C = 2 * DN_QK + DN_W
D_FF = 7168
N_EXPERTS = 8
TOP_K = 2
MOE_BLOCK = 256
N_DENSE = (DEPTH + 1) // 2
N_MOE = DEPTH // 2
ALPHA = (2.0 * DEPTH) ** 0.25
BETA_INIT = (8.0 * DEPTH) ** -0.25
EPS = 1e-6

kernel_name = 'hybrid_deltanet_gqa_gla_moe_encoder'


def rmsnorm(x, g):
    xf = x.astype(jnp.float32)
    y = xf * lax.rsqrt(jnp.mean(xf * xf, axis=-1, keepdims=True) + EPS)
    return (y * g.astype(jnp.float32)).astype(x.dtype)


def layernorm(x, g, b):
    xf = x.astype(jnp.float32)
    mu = jnp.mean(xf, axis=-1, keepdims=True)
    xc = xf - mu
    var = jnp.mean(xc * xc, axis=-1, keepdims=True)
    return (xc * lax.rsqrt(var + EPS) * g.astype(jnp.float32) + b.astype(jnp.float32)).astype(x.dtype)


def l2norm(x):
    xf = x.astype(jnp.float32)
    return (xf * lax.rsqrt(jnp.sum(xf * xf, axis=-1, keepdims=True) + EPS)).astype(x.dtype)


def to_heads(t, n_heads):
    b, s, _ = t.shape
    return t.reshape(b, s, n_heads, -1).transpose(0, 2, 1, 3)


def centred_short_conv(x, w):
    c = x.shape[-1]
    y = lax.conv_general_dilated(x, w[:, None, :].astype(x.dtype), window_strides=(1,),
                                 padding=[(CONV_W // 2, CONV_W // 2)],
                                 dimension_numbers=('NWC', 'WIO', 'NWC'),
                                 feature_group_count=c)
    return jax.nn.silu(y)


def chunk_first(t):
    return jnp.moveaxis(t, 2, 0)


def gated_delta_rule(q, k, v, g, beta):
    out_dtype = v.dtype
    f32 = jnp.float32
    z, h, s, dk = q.shape
    dv = v.shape[-1]
    c = DN_CHUNK
    n = s // c
    q = (q.astype(f32) * dk ** -0.5).reshape(z, h, n, c, dk)
    k = k.astype(f32).reshape(z, h, n, c, dk)
    v = v.astype(f32).reshape(z, h, n, c, dv)
    beta = beta.astype(f32).reshape(z, h, n, c)
    gc = jnp.cumsum(g.astype(f32).reshape(z, h, n, c), axis=-1)
    idx = jnp.arange(c)
    incl = idx[:, None] >= idx[None, :]
    strict = idx[:, None] > idx[None, :]
    decay = jnp.exp(jnp.where(incl, gc[..., :, None] - gc[..., None, :], -jnp.inf))
    kb = k * beta[..., None]
    m = jnp.where(strict, jnp.einsum('zhnid,zhnjd->zhnij', kb, k) * decay, 0.0)
    eye = jnp.eye(c, dtype=f32)
    t_inv = lax.linalg.triangular_solve(m + eye, jnp.broadcast_to(eye, m.shape), left_side=True,
                                        lower=True, unit_diagonal=True)
    u = t_inv @ (v * beta[..., None])
    w = t_inv @ (kb * jnp.exp(gc)[..., None])
    qk = jnp.einsum('zhnid,zhnjd->zhnij', q, k) * decay
    q_dec = q * jnp.exp(gc)[..., None]
    g_last = gc[..., -1]
    k_tail = k * jnp.exp(g_last[..., None] - gc)[..., None]

    def step(state, xs):
        u_n, w_n, qk_n, qd_n, kt_n, gl_n = xs
        v_new = u_n - w_n @ state
        o_n = qd_n @ state + qk_n @ v_new
        state = state * jnp.exp(gl_n)[..., None, None] + jnp.swapaxes(kt_n, -1, -2) @ v_new
        return state, o_n

    state0 = jnp.zeros((z, h, dk, dv), f32)
    xs = (chunk_first(u), chunk_first(w), chunk_first(qk), chunk_first(q_dec),
          chunk_first(k_tail), chunk_first(g_last))
    _, o = lax.scan(step, state0, xs)
    return jnp.moveaxis(o, 0, 2).reshape(z, h, s, dv).astype(out_dtype)


def gla_rule(q, k, v, gk):
    out_dtype = v.dtype
    f32 = jnp.float32
    z, h, s, dk = q.shape
    dv = v.shape[-1]
    c = GLA_CHUNK
    n = s // c
    q = (q.astype(f32) * dk ** -0.5).reshape(z, h, n, c, dk)
    k = k.astype(f32).reshape(z, h, n, c, dk)
    v = v.astype(f32).reshape(z, h, n, c, dv)
    gc = jnp.cumsum(gk.astype(f32).reshape(z, h, n, c, dk), axis=-2)
    q_dec = q * jnp.exp(gc)
    g_last = gc[..., -1, :]
    k_tail = k * jnp.exp(g_last[..., None, :] - gc)
    idx = jnp.arange(c)
    incl = (idx[:, None] >= idx[None, :])[..., None]

    def step(state, xs):
        q_n, k_n, v_n, g_n, qd_n, kt_n, gl_n = xs
        rel = jnp.exp(jnp.where(incl, g_n[..., :, None, :] - g_n[..., None, :, :], -jnp.inf))
        a = jnp.sum(q_n[..., :, None, :] * k_n[..., None, :, :] * rel, axis=-1)
        o_n = qd_n @ state + a @ v_n
        state = state * jnp.exp(gl_n)[..., :, None] + jnp.swapaxes(kt_n, -1, -2) @ v_n
        return state, o_n

    state0 = jnp.zeros((z, h, dk, dv), f32)
    xs = (chunk_first(q), chunk_first(k), chunk_first(v), chunk_first(gc),
          chunk_first(q_dec), chunk_first(k_tail), chunk_first(g_last))
    _, o = lax.scan(step, state0, xs)
    return jnp.moveaxis(o, 0, 2).reshape(z, h, s, dv).astype(out_dtype)


def axial_rope_tables(s):
    rows = s // GRID_W
    row = jnp.repeat(jnp.arange(rows, dtype=jnp.int32), GRID_W).astype(jnp.float32)
    col = jnp.tile(jnp.arange(GRID_W, dtype=jnp.int32), rows).astype(jnp.float32)
    inv_freq = ROPE_THETA ** (-jnp.arange(0, ROPE_SUB, 2, dtype=jnp.float32) / ROPE_SUB)
    ang_r = row[:, None] * inv_freq[None, :]
    ang_c = col[:, None] * inv_freq[None, :]
    return (jnp.cos(ang_r), jnp.sin(ang_r), jnp.cos(ang_c), jnp.sin(ang_c))


def rotate_sub(x, cos, sin):
    half = x.shape[-1] // 2
    x1, x2 = x[..., :half], x[..., half:]
    cos = cos.astype(x.dtype)
    sin = sin.astype(x.dtype)
    return jnp.concatenate([x1 * cos - x2 * sin, x2 * cos + x1 * sin], axis=-1)


def apply_axial_rope(x, tabs):
    cos_r, sin_r, cos_c, sin_c = tabs
    return jnp.concatenate([rotate_sub(x[..., :ROPE_SUB], cos_r, sin_r),
                            rotate_sub(x[..., ROPE_SUB:], cos_c, sin_c)], axis=-1)


def gqa_bidirectional(q, k, v):
    b, hq, s, dh = q.shape
    grp = hq // ATT_KV_HEADS
    nb = s // Q_BLOCK
    qb = jnp.moveaxis(q.reshape(b, ATT_KV_HEADS, grp, nb, Q_BLOCK, dh), 3, 0)
    scale = dh ** -0.5

    def block(qi):
        sc = jnp.einsum('bkgqd,bksd->bkgqs', qi, k).astype(jnp.float32) * scale
        p = jax.nn.softmax(sc, axis=-1).astype(v.dtype)
        return jnp.einsum('bkgqs,bksd->bkgqd', p, v)

    o = lax.map(block, qb)
    return jnp.moveaxis(o, 0, 3).reshape(b, hq, s, dh)


def hybrid_mixer(x, w_in, dn_conv, dn_a_log, dn_dt_bias, dn_norm_g, att_qn_g, att_kn_g,
                 gla_up, gla_up_b, gla_norm_g, w_out, rope):
    b, s, _ = x.shape
    proj = x @ w_in
    (dq, dk, dv, dgate, a_f, a_b, b_f, b_b, aq, ak, av,
     gq, gkk, gv, ggate, lr_f, lr_b) = jnp.split(proj, np.cumsum(IN_SIZES)[:-1].tolist(), axis=-1)

    def flip(t):
        return jnp.flip(t, axis=2)

    qkv = centred_short_conv(jnp.concatenate([dq, dk, dv], axis=-1), dn_conv)
    dq, dk, dv = jnp.split(qkv, [DN_QK, 2 * DN_QK], axis=-1)
    q = l2norm(to_heads(dq, DN_HEADS))
    k = l2norm(to_heads(dk, DN_HEADS))
    v = to_heads(dv, DN_HEADS)
    a = jnp.stack([a_f, a_b], axis=0)
    g = -jnp.exp(dn_a_log)[:, None, None, :] * jax.nn.softplus(a + dn_dt_bias[:, None, None, :])
    beta = jax.nn.sigmoid(jnp.stack([b_f, b_b], axis=0))
    g = g.transpose(0, 1, 3, 2)
    beta = beta.transpose(0, 1, 3, 2)
    o2 = gated_delta_rule(jnp.concatenate([q, flip(q)], axis=0),
                          jnp.concatenate([k, flip(k)], axis=0),
                          jnp.concatenate([v, flip(v)], axis=0),
                          jnp.concatenate([g[0], flip(g[1])], axis=0),
                          jnp.concatenate([beta[0], flip(beta[1])], axis=0))
    o_dn = (o2[:b] + flip(o2[b:])).transpose(0, 2, 1, 3)
    o_dn = rmsnorm(o_dn, dn_norm_g) * jax.nn.silu(dgate.reshape(b, s, DN_HEADS, DN_DV))
    o_dn = o_dn.reshape(b, s, DN_W)

    q = apply_axial_rope(rmsnorm(to_heads(aq, ATT_HEADS), att_qn_g), rope)
    k = apply_axial_rope(rmsnorm(to_heads(ak, ATT_KV_HEADS), att_kn_g), rope)
    v = to_heads(av, ATT_KV_HEADS)
    o_att = gqa_bidirectional(q, k, v).transpose(0, 2, 1, 3).reshape(b, s, ATT_W)

    q = to_heads(gq, GLA_HEADS)
    k = to_heads(gkk, GLA_HEADS)
    v = to_heads(gv, GLA_HEADS)
    lr = jnp.stack([lr_f, lr_b], axis=0)
    gk = jax.nn.log_sigmoid(jnp.einsum('zbsr,zrc->zbsc', lr, gla_up) + gla_up_b[:, None, None, :]) / GLA_NORMALIZER
    gk = gk.reshape(2, b, s, GLA_HEADS, GLA_DK).transpose(0, 1, 3, 2, 4)
    o2 = gla_rule(jnp.concatenate([q, flip(q)], axis=0),
                  jnp.concatenate([k, flip(k)], axis=0),
                  jnp.concatenate([v, flip(v)], axis=0),
                  jnp.concatenate([gk[0], flip(gk[1])], axis=0))
    o_gla = (o2[:b] + flip(o2[b:])).transpose(0, 2, 1, 3)
    o_gla = rmsnorm(o_gla, gla_norm_g) * jax.nn.silu(ggate.reshape(b, s, GLA_HEADS, GLA_DV))
    o_gla = o_gla.reshape(b, s, GLA_W)

    return jnp.concatenate([o_dn, o_att, o_gla], axis=-1) @ w_out


def swiglu(x, w_gate, w_up, w_down):
    return (jax.nn.silu(x @ w_gate) * (x @ w_up)) @ w_down


def moe_swiglu(x, router_w, w_gate, w_up, w_down):
    b, s, d = x.shape
    t = b * s
    xt = x.reshape(t, d)
    logits = (xt @ router_w).astype(jnp.float32)
    top_val, top_idx = lax.top_k(logits, TOP_K)
    gates = jax.nn.softmax(top_val, axis=-1)
    e_flat = top_idx.reshape(-1).astype(jnp.int32)
    tok_flat = jnp.repeat(jnp.arange(t, dtype=jnp.int32), TOP_K)
    gate_flat = gates.reshape(-1)
    order = jnp.argsort(e_flat)
    e_sorted = e_flat[order]
    tok_sorted = tok_flat[order]
    gate_sorted = gate_flat[order]
    counts = jax.ops.segment_sum(jnp.ones_like(e_flat), e_flat, num_segments=N_EXPERTS)
    padded = (counts + MOE_BLOCK - 1) // MOE_BLOCK * MOE_BLOCK
    start = jnp.cumsum(counts) - counts
    pstart = jnp.cumsum(padded) - padded
    pend = pstart + padded
    dest = pstart[e_sorted] + jnp.arange(TOP_K * t, dtype=jnp.int32) - start[e_sorted]
    nb = -(-(TOP_K * t) // MOE_BLOCK) + N_EXPERTS
    cap = nb * MOE_BLOCK
    buf_tok = jnp.zeros((cap,), jnp.int32).at[dest].set(tok_sorted)
    buf_gate = jnp.zeros((cap,), gates.dtype).at[dest].set(gate_sorted)
    block_start = jnp.arange(nb, dtype=jnp.int32) * MOE_BLOCK
    block_exp = jnp.minimum(jnp.sum(block_start[:, None] >= pend[None, :], axis=-1), N_EXPERTS - 1)
    xb = xt[buf_tok].reshape(nb, MOE_BLOCK, d)

    def expert_block(args):
        xi, e = args
        return swiglu(xi, w_gate[e], w_up[e], w_down[e])

    yb = lax.map(expert_block, (xb, block_exp)).reshape(cap, d)
    y = jax.ops.segment_sum(yb * buf_gate[:, None].astype(yb.dtype), buf_tok, num_segments=t)
    return y.reshape(b, s, d)


def setup_inputs(seed: int = 0) -> dict:
    key = jax.random.key(seed)
    ks = jax.random.split(key, 24)
    f32 = jnp.float32

    def nrm(k, shape, scale):
        return jax.random.normal(k, shape, f32) * scale

    x = nrm(ks[0], (BATCH, SEQ, D_MODEL), 1.0)
    w_in = nrm(ks[1], (DEPTH, D_MODEL, IN_COLS), D_MODEL ** -0.5)
    dn_conv = nrm(ks[2], (DEPTH, CONV_W, DN_CONV_C), CONV_W ** -0.5)
    dn_a_log = jnp.log(jax.random.uniform(ks[3], (DEPTH, 2, DN_HEADS), f32, minval=1.0, maxval=16.0))
    dt = jnp.exp(jax.random.uniform(ks[4], (DEPTH, 2, DN_HEADS), f32,
                                    minval=math.log(1e-3), maxval=math.log(1e-1)))
    dn_dt_bias = dt + jnp.log(-jnp.expm1(-dt))
    dn_norm_g = 1.0 + nrm(ks[5], (DEPTH, DN_DV), 0.02)
    att_qn_g = 1.0 + nrm(ks[6], (DEPTH, ATT_DH), 0.02)
    att_kn_g = 1.0 + nrm(ks[7], (DEPTH, ATT_DH), 0.02)
    gla_up = nrm(ks[8], (DEPTH, 2, GLA_RANK, GLA_QK), GLA_RANK ** -0.5)
    gla_up_b = nrm(ks[9], (DEPTH, 2, GLA_QK), 0.02)
    gla_norm_g = 1.0 + nrm(ks[10], (DEPTH, GLA_DV), 0.02)
    w_out = nrm(ks[11], (DEPTH, MIX_W, D_MODEL), BETA_INIT * MIX_W ** -0.5)
    ln1_g = 1.0 + nrm(ks[12], (DEPTH, D_MODEL), 0.02)
    ln1_b = nrm(ks[13], (DEPTH, D_MODEL), 0.02)
    ln2_g = 1.0 + nrm(ks[14], (DEPTH, D_MODEL), 0.02)
    ln2_b = nrm(ks[15], (DEPTH, D_MODEL), 0.02)
    ffn_w_gate = nrm(ks[16], (N_DENSE, D_MODEL, D_FF), D_MODEL ** -0.5)
    ffn_w_up = nrm(ks[17], (N_DENSE, D_MODEL, D_FF), D_MODEL ** -0.5)
    ffn_w_down = nrm(ks[18], (N_DENSE, D_FF, D_MODEL), BETA_INIT * D_FF ** -0.5)
    router_w = nrm(ks[19], (N_MOE, D_MODEL, N_EXPERTS), D_MODEL ** -0.5)
    exp_w_gate = nrm(ks[20], (N_MOE, N_EXPERTS, D_MODEL, D_FF), D_MODEL ** -0.5)
    exp_w_up = nrm(ks[21], (N_MOE, N_EXPERTS, D_MODEL, D_FF), D_MODEL ** -0.5)
    exp_w_down = nrm(ks[22], (N_MOE, N_EXPERTS, D_FF, D_MODEL), BETA_INIT * D_FF ** -0.5)
    return {'x': x, 'w_in': w_in, 'dn_conv': dn_conv, 'dn_a_log': dn_a_log, 'dn_dt_bias': dn_dt_bias,
            'dn_norm_g': dn_norm_g, 'att_qn_g': att_qn_g, 'att_kn_g': att_kn_g, 'gla_up': gla_up,
            'gla_up_b': gla_up_b, 'gla_norm_g': gla_norm_g, 'w_out': w_out, 'ln1_g': ln1_g,
            'ln1_b': ln1_b, 'ln2_g': ln2_g, 'ln2_b': ln2_b, 'ffn_w_gate': ffn_w_gate,
            'ffn_w_up': ffn_w_up, 'ffn_w_down': ffn_w_down, 'router_w': router_w,
            'exp_w_gate': exp_w_gate, 'exp_w_up': exp_w_up, 'exp_w_down': exp_w_down}


def reference(x, w_in, dn_conv, dn_a_log, dn_dt_bias, dn_norm_g, att_qn_g, att_kn_g, gla_up,
              gla_up_b, gla_norm_g, w_out, ln1_g, ln1_b, ln2_g, ln2_b, ffn_w_gate, ffn_w_up,
              ffn_w_down, router_w, exp_w_gate, exp_w_up, exp_w_down):
    rope = axial_rope_tables(x.shape[1])
    for layer in range(DEPTH):
        mix = hybrid_mixer(x, w_in[layer], dn_conv[layer], dn_a_log[layer], dn_dt_bias[layer],
                           dn_norm_g[layer], att_qn_g[layer], att_kn_g[layer], gla_up[layer],
                           gla_up_b[layer], gla_norm_g[layer], w_out[layer], rope)
        x = layernorm(ALPHA * x + mix, ln1_g[layer], ln1_b[layer])
        j = layer // 2
        if layer % 2 == 0:
            ffn = swiglu(x, ffn_w_gate[j], ffn_w_up[j], ffn_w_down[j])
        else:
            ffn = moe_swiglu(x, router_w[j], exp_w_gate[j], exp_w_up[j], exp_w_down[j])
        x = layernorm(ALPHA * x + ffn, ln2_g[layer], ln2_b[layer])
    return x
```

```python
import os


import numpy as np
import concourse.bass as bass
import concourse.mybir as mybir
from concourse.bass_utils import run_bass_kernel_spmd
from contextlib import ExitStack

F32 = mybir.dt.float32
BF16 = mybir.dt.bfloat16
I32 = mybir.dt.int32
AF = mybir.ActivationFunctionType
ALU = mybir.AluOpType
AX = mybir.AxisListType


class Buf:
    __slots__ = ("t", "w", "r", "name", "dsem")

    def __init__(self, t, name):
        self.t = t
        self.name = name
        self.w = None
        self.r = {}
        self.dsem = None

    def __getitem__(self, idx):
        return self.t[idx]


class Reg:
    __slots__ = ("b", "c0", "name")

    def __init__(self, b, c0, name):
        self.b = b
        self.c0 = c0
        self.name = name

    def __getitem__(self, idx):
        rs, cs = idx
        a = self.c0 + (cs.start or 0)
        z = self.c0 + cs.stop
        return self.b.t[rs, a:z]

    w = property(lambda self: self.b.w, lambda self, v: setattr(self.b, "w", v))
    r = property(lambda self: self.b.r, lambda self, v: setattr(self.b, "r", v))
    dsem = property(lambda self: self.b.dsem, lambda self, v: setattr(self.b, "dsem", v))


class PB:
    ENGS = ("tensor", "vector", "scalar", "gpsimd", "sync")

    def __init__(self, same_engine_sync=True):
        self.nc = bass.Bass("TRN2", target_bir_lowering=False)
        self.es = ExitStack()
        self.base_es = self.es
        self.scopes = []
        self.streams = {e: [] for e in self.ENGS}
        self.sems = {}
        self.semcount = {}
        self.known = {e: {} for e in self.ENGS}
        self.same_engine_sync = same_engine_sync
        for e in ("tensor", "vector", "scalar", "gpsimd"):
            self._mksem("E_" + e)
        self.nbuf = 0
        self.nops = 0
        import os
        self.limit = int(os.environ.get('PB_LIMIT', '1000000000'))
        self.trace_op = int(os.environ.get('PB_TRACE', '-1'))
        self.nopool = os.environ.get('PB_NOPOOL') == '1'
        self.ninstr = {}

    def _mksem(self, key):
        self.sems[key] = self.base_es.enter_context(self.nc.semaphore(key))
        self.semcount[key] = 0

    def sb(self, name, shape, dtype):
        t = self.es.enter_context(self.nc.sbuf_tensor(name, list(shape), dtype))
        return Buf(t, name)

    def ps(self, name, shape, dtype=F32):
        t = self.es.enter_context(self.nc.psum_tensor(name, list(shape), dtype))
        return Buf(t, name)

    def dram(self, name, shape, dtype, kind="Internal"):
        t = self.nc.dram_tensor(name, list(shape), dtype, kind=kind)
        return Buf(t.ap(), name)

    def view(self, buf_or_ap, name=None):
        self.nbuf += 1
        return Buf(buf_or_ap, name or f"v{self.nbuf}")

    def push(self):
        self.scopes.append(self.es)
        self.es = ExitStack()

    def pop(self):
        for eng in self.ENGS:
            e = getattr(self.nc, eng)
            kn = self.known[eng]
            for k, v in self.semcount.items():
                if v > 0 and kn.get(k, 0) < v:
                    e.wait_ge(self.sems[k], v)
                    kn[k] = v
        self.es.close()
        self.es = self.scopes.pop()

    def _deps(self, stream, reads, writes, skipkey=None):
        need = {}
        def add(k, v):
            if v > need.get(k, 0):
                need[k] = v
        for b in reads:
            if b.w is not None:
                add(*b.w)
        for b in writes:
            if b.w is not None and b.w[0] != skipkey:
                add(*b.w)
            for k, v in b.r.items():
                add(k, v)
        own = "E_" + stream
        kn = self.known[stream]
        out = []
        for k, v in need.items():
            if k == own and (not self.same_engine_sync or stream == "tensor"):
                continue
            if kn.get(k, 0) >= v:
                continue
            kn[k] = v
            out.append((k, v))
        return out

    def op(self, eng, fn, reads=(), writes=(), inc=True):
        self.nops += 1
        if self.nops > self.limit:
            return 0
        if self.nops == self.trace_op:
            import traceback; traceback.print_stack(limit=4)
        if eng == 'gpsimd' and self.nopool:
            eng = 'vector'
        waits = self._deps(eng, reads, writes)
        key = "E_" + eng
        if inc:
            self.semcount[key] += 1
            val = self.semcount[key]
        else:
            val = self.semcount[key] + 1
        self._emit(eng, waits, fn, key, 1 if inc else 0)
        for b in reads:
            if b.r.get(key, 0) < val:
                b.r[key] = val
        for b in writes:
            b.w = (key, val)
            b.r = {}
        return val

    def _emit(self, eng, waits, fn, key, inc):
        e = getattr(self.nc, eng)
        for k, v in waits:
            e.wait_ge(self.sems[k], v)
        self.ninstr[eng] = self.ninstr.get(eng, 0) + 1
        if fn is not None:
            ins = fn(e)
            if inc:
                ins.then_inc(self.sems[key], inc)

    def dma(self, queue, out, in_, reads=(), writes=(), sembuf=None, **kw):
        self.nops += 1
        if self.nops > self.limit:
            return 0
        if self.nops == self.trace_op:
            import traceback; traceback.print_stack(limit=4)
        sb_ = sembuf or (writes[0] if writes else reads[0])
        if sb_.dsem is None:
            sb_.dsem = "D_%d_%s" % (len(self.sems), sb_.name)
            self._mksem(sb_.dsem)
        key = sb_.dsem
        waits = self._deps(queue, reads, writes, skipkey=key)
        self.semcount[key] += 16
        val = self.semcount[key]
        fn = lambda e, out=out, in_=in_, kw=kw: e.dma_start(out=out, in_=in_, **kw)
        self._emit(queue, waits, fn, key, 16)
        for b in reads:
            if b.r.get(key, 0) < val:
                b.r[key] = val
        for b in writes:
            b.w = (key, val)
            b.r = {}
        return val

    def finish(self):
        self.final_waits = [(k, v) for k, v in self.semcount.items() if v > 0]

    def emit(self):
        nc = self.nc
        sems = self.sems
        fw_ = self.final_waits
        with nc.Block() as block:
            def run(e, name):
                for k, v in fw_:
                    e.wait_ge(sems[k], v)

            @block.tensor
            def _(e):
                run(e, "tensor")

            @block.vector
            def _(e):
                run(e, "vector")

            @block.scalar
            def _(e):
                run(e, "scalar")

            @block.gpsimd
            def _(e):
                run(e, "gpsimd")

            @block.sync
            def _(e):
                run(e, "sync")
        self.base_es.close()
        return nc


FM1 = 864
TM1 = 456
FMO = dict(qA=(0,128), kA=(128,128), vA=(256,128), qB=(384,128), kB=(512,128), vB=(640,64),
           gq=(704,64), gk=(768,64), lrf=(832,16), lrb=(848,16))
CN = ["IDENT", "ONES", "NEGONES", "TRI0", "TRI1", "NEGM0", "NEGM1", "STR0", "STR1", "GM0", "GM1", "ROT"]
EPS = 1e-6
import os
EVAC_ACT = True


def make_consts():
    i = np.arange(128)
    c = {}
    c["IDENT"] = np.eye(128)
    c["ONES"] = np.ones((128, 128))
    c["NEGONES"] = -np.ones((128, 128))
    c["TRI0"] = (i[:, None] <= i[None, :]).astype(np.float64)
    c["TRI1"] = (i[:, None] >= i[None, :]).astype(np.float64)
    c["NEGM0"] = np.where(i[:, None] >= i[None, :], 0.0, -30000.0)
    c["NEGM1"] = np.where(i[:, None] <= i[None, :], 0.0, -30000.0)
    c["STR0"] = (i[:, None] > i[None, :]).astype(np.float64)
    c["STR1"] = (i[:, None] < i[None, :]).astype(np.float64)
    c["GM0"] = (i[None, :] >= i[:, None]).astype(np.float64)
    c["GM1"] = (i[None, :] <= i[:, None]).astype(np.float64)
    rot = np.zeros((128, 128))
    for m in range(128):
        if (m % 64) < 32:
            rot[m + 32, m] = -1.0
        else:
            rot[m - 32, m] = 1.0
    c["ROT"] = rot
    return np.concatenate([c[n] for n in CN], axis=1).astype(np.float32)


class Ring:
    def __init__(self, bufs):
        self.bufs = bufs
        self.i = 0

    def next(self):
        b = self.bufs[self.i % len(self.bufs)]
        self.i += 1
        return b


def phase_dngla(p, S, xdt, xb, wfm, wtm, prm, glaup, cst, oA, oB, ogl, gts, ofw, chdt=BF16, scdt=BF16, predt=BF16, banks=None):
    nc = p.nc
    NT = S // 128
    NB = S // 256
    wfm_sb = p.sb("wfm_sb", [128, 16, FM1], BF16)
    wtm_sb = p.sb("wtm_sb", [128, 16, TM1], BF16)
    prm_sb = p.sb("prm_sb", [128, 64], F32)
    up_sb = p.sb("up_sb", [16, 128], BF16)
    cst_sb = p.sb("cst_sb", [128, len(CN) * 128], F32)
    idb = p.sb("idb", [128, 128], BF16)
    idf = p.sb("idf", [128, 128], F32)
    IDN = {BF16: idb, F32: idf}
    C = {n: cst_sb[:, i * 128:(i + 1) * 128] for i, n in enumerate(CN)}
    for k in range(16):
        p.dma("gpsimd", wfm_sb[:, k, :], wfm[:, k * FM1:(k + 1) * FM1], writes=[wfm_sb])
        p.dma("gpsimd", wtm_sb[:, k, :], wtm[:, k * TM1:(k + 1) * TM1], writes=[wtm_sb])
    p.dma("sync", prm_sb[:], prm[:, :], writes=[prm_sb])
    p.dma("gpsimd", up_sb[:], glaup[:, :], writes=[up_sb])
    p.dma("sync", cst_sb[:], cst[:, :], writes=[cst_sb])
    p.op("vector", lambda e: e.tensor_copy(out=idb[:], in_=C["IDENT"]), reads=[cst_sb], writes=[idb])
    p.op("vector", lambda e: e.tensor_copy(out=idf[:], in_=C["IDENT"]), reads=[cst_sb], writes=[idf])
    expA = p.sb("expA", [128, 4], F32)
    p.op("scalar", lambda e: e.activation(out=expA[:], in_=prm_sb[:, 30:34], func=AF.Exp), reads=[prm_sb], writes=[expA])

    if banks is None:
        banks = [p.ps(f"bank{i}", [128, 512], F32) for i in range(8)]
    PR = Ring([banks[0], banks[1], banks[2]])
    qregs = []
    for b in (3, 4, 5):
        for q in range(4):
            qregs.append(Reg(banks[b], q * 128, f"q{b}_{q}"))
    QR = Ring(qregs)
    hregs = []
    for b in (6, 7):
        for h in range(2):
            hregs.append(Reg(banks[b], h * 256, f"h{b}_{h}"))
    HR = Ring(hregs)

    ofw_t = [p.view(ofw[t * 128:(t + 1) * 128, :], f'ofw{t}') for t in range(NT)]
    evac_i = [0]

    def evac(out_buf, out_ap, in_buf, in_ap, extra_reads=()):
        evac_i[0] += 1
        if evac_i[0] % 2 or EVAC_ACT:
            p.op("scalar", lambda e: e.copy(out=out_ap, in_=in_ap), reads=[in_buf, *extra_reads], writes=[out_buf])
        else:
            p.op("vector", lambda e: e.tensor_scalar(out=out_ap, in0=in_ap, scalar1=1.0, scalar2=None, op0=ALU.mult), reads=[in_buf, *extra_reads], writes=[out_buf])

    def transp(src_buf, src_ap, M, dst_buf, dst_ap):
        q = QR.next()
        idc = IDN[src_buf.t.dtype]
        p.op("tensor", lambda e: e.matmul(q[0:M, 0:128], lhsT=src_ap, rhs=idc[:], start=True, stop=True),
             reads=[src_buf, idc], writes=[q])
        evac(dst_buf, dst_ap, q, q[0:M, 0:128])

    slots = [dict(name="A", q="qA", k="kA", v="vA", dvw=128, ci=0, oc=0),
             dict(name="B", q="qB", k="kB", v="vB", dvw=64, ci=3, oc=128)]
    CH = []
    for tt in range(2):
        for s in slots:
            n = f"{tt}{s['name']}"
            dvw = s["dvw"]
            ch = dict(tt=tt, s=s, dvw=dvw)
            for nm, shp, dt in [("qn", [128, 128], predt), ("kn", [128, 128], predt), ("vt", [128, dvw], predt),
                                ("kT", [128, 128], predt), ("qT", [128, 128], predt), ("G1", [128, 128], F32),
                                ("Dm", [128, 128], F32), ("D", [128, 128], F32), ("t1", [128, 128], F32),
                                ("N0", [128, 128], chdt), ("N1", [128, 128], chdt), ("NT0", [128, 128], chdt),
                                ("NT1", [128, 128], chdt), ("R0", [128, dvw + 128], chdt), ("R1", [128, dvw + 128], chdt),
                                ("u", [128, dvw], F32), ("w", [128, 128], scdt), ("wT", [128, 128], scdt),
                                ("qkD", [128, 128], scdt), ("qkDT", [128, 128], scdt), ("qd", [128, 128], scdt),
                                ("qdT", [128, 128], scdt), ("kt", [128, 128], scdt), ("vn", [128, dvw], scdt),
                                ("ss", [128, 4], F32), ("osb", [128, dvw], F32)]:
                ch[nm] = p.sb(f"{nm}_{n}", shp, dt)
            CH.append(ch)
    for s in slots:
        s["S"] = p.sb(f"S_{s['name']}", [128, s["dvw"]], F32)
        s["Sb"] = p.sb(f"Sb_{s['name']}", [128, s["dvw"]], scdt)
        s["cq"] = p.sb(f"cq_{s['name']}", [128, 256], predt)
        s["ck"] = p.sb(f"ck_{s['name']}", [128, 256], predt)
        s["cv"] = p.sb(f"cv_{s['name']}", [128, 256], predt)
        s["acc"] = p.sb(f"acc_{s['name']}", [128, 256], F32)
    SC = []
    for tt in range(2):
        d = {}
        for nm in ["z", "t", "e", "l", "g", "beta", "gc", "gl", "egc", "etail", "egl", "nbeta", "bege", "tmp"]:
            d[nm] = p.sb(f"sc_{nm}{tt}", [128, 2], F32)
        d["tm"] = p.sb(f"tm{tt}", [128, TM1], F32)
        d["gvb"] = p.sb(f"gvb{tt}", [128, 128], BF16)
        d["ofl"] = p.sb(f"ofl{tt}", [128, 320], F32)
        SC.append(d)
    xblk = [p.sb(f"xblk{i}", [128, 16, 260], BF16) for i in range(2)]
    G = {}
    for nm, shp, dt in [("q", [64, 256], F32), ("k", [64, 256], F32), ("lr0", [16, 256], BF16), ("lr1", [16, 256], BF16),
                        ("zb", [64, 256], F32), ("ta", [64, 256], F32), ("gkk", [64, 256], F32), ("gc", [64, 256], F32),
                        ("ex", [64, 256], F32), ("qpos", [64, 256], BF16), ("kneg", [64, 256], BF16),
                        ("ktl", [64, 256], BF16), ("ones", [64, 256], F32), ("glc", [64, 2], F32), ("egl", [64, 2], F32),
                        ("S", [64, 128], F32), ("Sb", [64, 128], BF16)]:
        G[nm] = p.sb(f"gla_{nm}", shp, dt)
    GT = []
    for tt in range(2):
        d = {}
        for nm, shp, dt in [("AT", [128, 128], BF16), ("kt", [128, 64], BF16), ("osb", [128, 128], F32)]:
            d[nm] = p.sb(f"glat_{nm}{tt}", shp, dt)
        GT.append(d)
    p.op("gpsimd", lambda e: e.memset(G["ones"][:], 1.0), writes=[G["ones"]])

    def fm_proj(blk, name):
        m0, M = FMO[name]
        bank = PR.next()
        for k in range(16):
            p.op("tensor", lambda e, k=k: e.matmul(bank[0:M, 0:260], lhsT=wfm_sb[:, k, m0:m0 + M], rhs=blk[:, k, :],
                                                   start=(k == 0), stop=(k == 15)),
                 reads=[wfm_sb, blk], writes=[bank], inc=(k == 15))
        return bank, M

    for dirn in (0, 1):
        TRI = C[f"TRI{dirn}"]; NEGM = C[f"NEGM{dirn}"]; STR = C[f"STR{dirn}"]; GM = C[f"GM{dirn}"]
        for s in slots:
            p.op("gpsimd", lambda e, s=s: e.memset(s["S"][:], 0.0), writes=[s["S"]])
            p.op("gpsimd", lambda e, s=s: e.memset(s["Sb"][:], 0.0), writes=[s["Sb"]])
        p.op("gpsimd", lambda e: e.memset(G["S"][:], 0.0), writes=[G["S"]])
        p.op("gpsimd", lambda e: e.memset(G["Sb"][:], 0.0), writes=[G["Sb"]])
        blocks = range(NB) if dirn == 0 else range(NB - 1, -1, -1)
        for bi, b in enumerate(blocks):
            blk = xblk[bi % 2]
            if xdt == BF16:
                p.dma("sync", blk[:].rearrange("p k t -> p (k t)"), xb[b, :, :], writes=[blk])
            else:
                for k in range(16):
                    p.dma("gpsimd", blk[:, k, :], xb[b, :, k * 260:(k + 1) * 260], writes=[blk])
            tiles = (0, 1) if dirn == 0 else (1, 0)
            ncol = TM1 if dirn == 0 else 136
            for tt in (0, 1):
                bank = PR.next()
                for k in range(16):
                    p.op("tensor", lambda e, k=k, tt=tt, bank=bank: e.matmul(
                        bank[:, 0:ncol], lhsT=blk[:, k, 2 + 128 * tt:2 + 128 * (tt + 1)], rhs=wtm_sb[:, k, 0:ncol],
                        start=(k == 0), stop=(k == 15)), reads=[blk, wtm_sb], writes=[bank], inc=(k == 15))
                tm = SC[tt]["tm"]
                evac(tm, tm[:, 0:ncol], bank, bank[:, 0:ncol])
                p.op("gpsimd", lambda e, tt=tt, tm=tm: e.tensor_copy(out=SC[tt]["gvb"][:], in_=tm[:, 0:128]),
                     reads=[tm], writes=[SC[tt]["gvb"]])
                if dirn == 0:
                    t0 = b * 256 + tt * 128
                    p.dma("sync", gts[t0:t0 + 128, :], tm[:, 136:456], reads=[tm], sembuf=tm)
            for s in slots:
                for comp, dst in (("q", "cq"), ("k", "ck"), ("v", "cv")):
                    bank, M = fm_proj(blk, s[comp])
                    ti = s["ci"] + ("q", "k", "v").index(comp)
                    acc = s["acc"]
                    p.op("vector", lambda e, bank=bank, M=M, ti=ti, acc=acc: e.tensor_scalar(
                        out=acc[0:M, :], in0=bank[0:M, 0:256], scalar1=prm_sb[0:M, 5 * ti:5 * ti + 1], scalar2=None,
                        op0=ALU.mult), reads=[bank, prm_sb], writes=[acc])
                    for j in range(1, 5):
                        p.op("vector", lambda e, bank=bank, M=M, ti=ti, acc=acc, j=j: e.scalar_tensor_tensor(
                            out=acc[0:M, :], in0=bank[0:M, j:j + 256], scalar=prm_sb[0:M, 5 * ti + j:5 * ti + j + 1],
                            in1=acc[0:M, :], op0=ALU.mult, op1=ALU.add), reads=[bank, prm_sb, acc], writes=[acc])
                    d_ = s[dst]
                    p.op("scalar", lambda e, d_=d_, M=M, acc=acc: e.activation(out=d_[0:M, :], in_=acc[0:M, :], func=AF.Silu),
                         reads=[acc], writes=[d_])
            for tt in (0, 1):
                sc = SC[tt]; tm = sc["tm"]
                a_ap = tm[:, 128 + 4 * dirn:128 + 4 * dirn + 2]
                b_ap = tm[:, 128 + 4 * dirn + 2:128 + 4 * dirn + 4]
                dtb = prm_sb[:, 34 + 2 * dirn:36 + 2 * dirn]
                Aex = expA[:, 2 * dirn:2 * dirn + 2]
                p.op("vector", lambda e, sc=sc, a_ap=a_ap, dtb=dtb: e.tensor_tensor(out=sc["z"][:], in0=a_ap, in1=dtb, op=ALU.add),
                     reads=[tm, prm_sb], writes=[sc["z"]])
                p.op("scalar", lambda e, sc=sc: e.activation(out=sc["t"][:], in_=sc["z"][:], func=AF.Abs), reads=[sc["z"]], writes=[sc["t"]])
                p.op("scalar", lambda e, sc=sc: e.activation(out=sc["e"][:], in_=sc["t"][:], func=AF.Exp, scale=-1.0), reads=[sc["t"]], writes=[sc["e"]])
                p.op("scalar", lambda e, sc=sc: e.activation(out=sc["l"][:], in_=sc["e"][:], func=AF.Ln, bias=1.0), reads=[sc["e"]], writes=[sc["l"]])
                p.op("vector", lambda e, sc=sc: e.scalar_tensor_tensor(out=sc["tmp"][:], in0=sc["z"][:], scalar=0.0, in1=sc["l"][:], op0=ALU.max, op1=ALU.add),
                     reads=[sc["z"], sc["l"]], writes=[sc["tmp"]])
                p.op("vector", lambda e, sc=sc, Aex=Aex: e.scalar_tensor_tensor(out=sc["g"][:], in0=sc["tmp"][:], scalar=-1.0, in1=Aex, op0=ALU.mult, op1=ALU.mult),
                     reads=[sc["tmp"], expA], writes=[sc["g"]])
                p.op("scalar", lambda e, sc=sc, b_ap=b_ap: e.activation(out=sc["beta"][:], in_=b_ap, func=AF.Sigmoid), reads=[tm], writes=[sc["beta"]])
                q1 = QR.next()
                p.op("tensor", lambda e, q1=q1, sc=sc: e.matmul(q1[:, 0:2], lhsT=TRI, rhs=sc["g"][:], start=True, stop=True), reads=[cst_sb, sc["g"]], writes=[q1])
                q2 = QR.next()
                p.op("tensor", lambda e, q2=q2, sc=sc: e.matmul(q2[:, 0:2], lhsT=C["ONES"], rhs=sc["g"][:], start=True, stop=True), reads=[cst_sb, sc["g"]], writes=[q2])
                p.op("vector", lambda e, q1=q1, sc=sc: e.tensor_copy(out=sc["gc"][:], in_=q1[:, 0:2]), reads=[q1], writes=[sc["gc"]])
                p.op("vector", lambda e, q2=q2, sc=sc: e.tensor_copy(out=sc["gl"][:], in_=q2[:, 0:2]), reads=[q2], writes=[sc["gl"]])
                p.op("scalar", lambda e, sc=sc: e.activation(out=sc["egc"][:], in_=sc["gc"][:], func=AF.Exp), reads=[sc["gc"]], writes=[sc["egc"]])
                p.op("scalar", lambda e, sc=sc: e.activation(out=sc["egl"][:], in_=sc["gl"][:], func=AF.Exp), reads=[sc["gl"]], writes=[sc["egl"]])
                p.op("vector", lambda e, sc=sc: e.tensor_tensor(out=sc["tmp"][:], in0=sc["gl"][:], in1=sc["gc"][:], op=ALU.subtract), reads=[sc["gl"], sc["gc"]], writes=[sc["tmp"]])
                p.op("scalar", lambda e, sc=sc: e.activation(out=sc["etail"][:], in_=sc["tmp"][:], func=AF.Exp), reads=[sc["tmp"]], writes=[sc["etail"]])
                p.op("vector", lambda e, sc=sc: e.tensor_scalar(out=sc["nbeta"][:], in0=sc["beta"][:], scalar1=-1.0, scalar2=None, op0=ALU.mult), reads=[sc["beta"]], writes=[sc["nbeta"]])
                p.op("vector", lambda e, sc=sc: e.tensor_tensor(out=sc["bege"][:], in0=sc["beta"][:], in1=sc["egc"][:], op=ALU.mult), reads=[sc["beta"], sc["egc"]], writes=[sc["bege"]])
            chs = CH
            def col(sc, nm, si):
                return sc[nm][:, si:si + 1]
            for ch in chs:
                s = ch["s"]; tt = ch["tt"]; dvw = ch["dvw"]
                for src, dstn, M in ((s["cq"], "qn", 128), (s["ck"], "kn", 128), (s["cv"], "vt", dvw)):
                    q = QR.next()
                    Mi = 128 if dstn != "vt" else dvw
                    p.op("tensor", lambda e, q=q, src=src, tt=tt, Mi=Mi: e.matmul(q[:, 0:Mi], lhsT=src[0:Mi, 128 * tt:128 * tt + 128], rhs=IDN[predt][0:Mi, 0:Mi], start=True, stop=True),
                         reads=[src, IDN[predt]], writes=[q])
                    if dstn == "vt":
                        evac(ch["vt"], ch["vt"][:], q, q[:, 0:dvw])
                    else:
                        ci = 0 if dstn == "qn" else 1
                        p.op("scalar", lambda e, ch=ch, q=q, ci=ci: e.activation(out=ch["t1"][:], in_=q[:, 0:128], func=AF.Square, accum_out=ch["ss"][:, ci:ci + 1]),
                             reads=[q], writes=[ch["t1"], ch["ss"]])
                        p.op("scalar", lambda e, ch=ch, ci=ci: e.activation(out=ch["ss"][:, ci + 2:ci + 3], in_=ch["ss"][:, ci:ci + 1], func=AF.Sqrt, bias=EPS, scale=1.0),
                             reads=[ch["ss"]], writes=[ch["ss"]])
                        p.op("vector", lambda e, ch=ch, ci=ci: e.reciprocal(out=ch["ss"][:, ci:ci + 1], in_=ch["ss"][:, ci + 2:ci + 3]),
                             reads=[ch["ss"]], writes=[ch["ss"]])
                        sc2 = (128.0 ** -0.5) if dstn == "qn" else 1.0
                        p.op("vector", lambda e, ch=ch, q=q, ci=ci, dstn=dstn, sc2=sc2: e.tensor_scalar(
                            out=ch[dstn][:], in0=q[:, 0:128], scalar1=ch["ss"][:, ci:ci + 1], scalar2=sc2, op0=ALU.mult, op1=ALU.mult),
                            reads=[q, ch["ss"]], writes=[ch[dstn]])
            for ch in chs:
                transp(ch["kn"], ch["kn"][:], 128, ch["kT"], ch["kT"][:])
                transp(ch["qn"], ch["qn"][:], 128, ch["qT"], ch["qT"][:])
            for ch in chs:
                sc = SC[ch["tt"]]; si = 0 if ch["s"]["name"] == "A" else 1
                p.op("gpsimd", lambda e, ch=ch, sc=sc, si=si: e.tensor_scalar(out=ch["G1"][:], in0=TRI, scalar1=sc["g"][:, si:si + 1], scalar2=None, op0=ALU.mult),
                     reads=[cst_sb, sc["g"]], writes=[ch["G1"]])
            for ch in chs:
                q = QR.next(); ch["qE"] = q
                p.op("tensor", lambda e, ch=ch, q=q: e.matmul(q[:, 0:128], lhsT=ch["G1"][:], rhs=C["ONES"], start=True, stop=False), reads=[ch["G1"], cst_sb], writes=[q])
                p.op("tensor", lambda e, ch=ch, q=q: e.matmul(q[:, 0:128], lhsT=C["NEGONES"], rhs=ch["G1"][:], start=False, stop=True), reads=[ch["G1"], cst_sb], writes=[q])
            for ch in chs:
                q = ch["qE"]
                p.op("vector", lambda e, ch=ch, q=q: e.scalar_tensor_tensor(out=ch["Dm"][:], in0=q[:, 0:128], scalar=0.0, in1=NEGM, op0=ALU.min, op1=ALU.add),
                     reads=[q, cst_sb], writes=[ch["Dm"]])
                p.op("scalar", lambda e, ch=ch: e.activation(out=ch["D"][:], in_=ch["Dm"][:], func=AF.Exp), reads=[ch["Dm"]], writes=[ch["D"]])
            for ch in chs:
                sc = SC[ch["tt"]]; si = 0 if ch["s"]["name"] == "A" else 1
                q = QR.next()
                p.op("tensor", lambda e, ch=ch, q=q: e.matmul(q[:, 0:128], lhsT=ch["kT"][:], rhs=ch["kT"][:], start=True, stop=True), reads=[ch["kT"]], writes=[q])
                p.op("vector", lambda e, ch=ch, q=q: e.tensor_tensor(out=ch["t1"][:], in0=q[:, 0:128], in1=ch["D"][:], op=ALU.mult), reads=[q, ch["D"]], writes=[ch["t1"]])
                p.op("vector", lambda e, ch=ch, sc=sc, si=si: e.scalar_tensor_tensor(out=ch["N0"][:], in0=ch["t1"][:], scalar=sc["nbeta"][:, si:si + 1], in1=STR, op0=ALU.mult, op1=ALU.mult),
                     reads=[ch["t1"], sc["nbeta"], cst_sb], writes=[ch["N0"]])
                q2 = QR.next()
                p.op("tensor", lambda e, ch=ch, q2=q2: e.matmul(q2[:, 0:128], lhsT=ch["qT"][:], rhs=ch["kT"][:], start=True, stop=True), reads=[ch["qT"], ch["kT"]], writes=[q2])
                p.op("vector", lambda e, ch=ch, q2=q2: e.tensor_tensor(out=ch["qkD"][:], in0=q2[:, 0:128], in1=ch["D"][:], op=ALU.mult), reads=[q2, ch["D"]], writes=[ch["qkD"]])
            for ch in chs:
                transp(ch["N0"], ch["N0"][:], 128, ch["NT0"], ch["NT0"][:])
                transp(ch["qkD"], ch["qkD"][:], 128, ch["qkDT"], ch["qkDT"][:])
            for ch in chs:
                sc = SC[ch["tt"]]; si = 0 if ch["s"]["name"] == "A" else 1; dvw = ch["dvw"]
                p.op("gpsimd", lambda e, ch=ch, sc=sc, si=si, dvw=dvw: e.tensor_scalar(out=ch["R0"][:, 0:dvw], in0=ch["vt"][:], scalar1=sc["beta"][:, si:si + 1], scalar2=None, op0=ALU.mult),
                     reads=[ch["vt"], sc["beta"]], writes=[ch["R0"]])
                p.op("gpsimd", lambda e, ch=ch, sc=sc, si=si, dvw=dvw: e.tensor_scalar(out=ch["R0"][:, dvw:dvw + 128], in0=ch["kn"][:], scalar1=sc["bege"][:, si:si + 1], scalar2=None, op0=ALU.mult),
                     reads=[ch["kn"], sc["bege"], ch["R0"]], writes=[ch["R0"]])
                p.op("gpsimd", lambda e, ch=ch, sc=sc, si=si: e.tensor_scalar(out=ch["kt"][:], in0=ch["kn"][:], scalar1=sc["etail"][:, si:si + 1], scalar2=None, op0=ALU.mult),
                     reads=[ch["kn"], sc["etail"]], writes=[ch["kt"]])
                p.op("gpsimd", lambda e, ch=ch, sc=sc, si=si: e.tensor_scalar(out=ch["qd"][:], in0=ch["qn"][:], scalar1=sc["egc"][:, si:si + 1], scalar2=None, op0=ALU.mult),
                     reads=[ch["qn"], sc["egc"]], writes=[ch["qd"]])
            for ch in chs:
                transp(ch["qd"], ch["qd"][:], 128, ch["qdT"], ch["qdT"][:])
            for lev in range(7):
                a, bnx = lev % 2, (lev + 1) % 2
                for ch in chs:
                    dvw = ch["dvw"]; W = dvw + 128
                    Nk, NTk = ch[f"N{a}"], ch[f"NT{a}"]
                    Rk, Rn = ch[f"R{a}"], ch[f"R{bnx}"]
                    h = HR.next()
                    p.op("tensor", lambda e, h=h, NTk=NTk, Rk=Rk, W=W: e.matmul(h[:, 0:W], lhsT=NTk[:], rhs=Rk[:, 0:W], start=True, stop=True), reads=[NTk, Rk], writes=[h])
                    if lev < 6:
                        p.op("vector", lambda e, h=h, Rk=Rk, Rn=Rn, W=W: e.tensor_tensor(out=Rn[:, 0:W], in0=h[:, 0:W], in1=Rk[:, 0:W], op=ALU.add), reads=[h, Rk], writes=[Rn])
                        qa = QR.next(); qb = QR.next()
                        p.op("tensor", lambda e, qa=qa, Nk=Nk, NTk=NTk: e.matmul(qa[:, 0:128], lhsT=NTk[:], rhs=Nk[:], start=True, stop=True), reads=[Nk, NTk], writes=[qa])
                        p.op("tensor", lambda e, qb=qb, Nk=Nk, NTk=NTk: e.matmul(qb[:, 0:128], lhsT=Nk[:], rhs=NTk[:], start=True, stop=True), reads=[Nk, NTk], writes=[qb])
                        evac(ch[f"N{bnx}"], ch[f"N{bnx}"][:], qa, qa[:, 0:128])
                        evac(ch[f"NT{bnx}"], ch[f"NT{bnx}"][:], qb, qb[:, 0:128])
                    else:
                        p.op("vector", lambda e, h=h, Rk=Rk, ch=ch, dvw=dvw: e.tensor_tensor(out=ch["u"][:], in0=h[:, 0:dvw], in1=Rk[:, 0:dvw], op=ALU.add), reads=[h, Rk], writes=[ch["u"]])
                        p.op("vector", lambda e, h=h, Rk=Rk, ch=ch, dvw=dvw: e.tensor_tensor(out=ch["w"][:], in0=h[:, dvw:dvw + 128], in1=Rk[:, dvw:dvw + 128], op=ALU.add), reads=[h, Rk], writes=[ch["w"]])
            for ch in chs:
                transp(ch["w"], ch["w"][:], 128, ch["wT"], ch["wT"][:])
            for nm in ("gq", "gk"):
                bank, M = fm_proj(blk, nm)
                dst = G["q"] if nm == "gq" else G["k"]
                evac(dst, dst[:], bank, bank[0:64, 2:258])
            lrn = "lrf" if dirn == 0 else "lrb"
            bank, M = fm_proj(blk, lrn)
            lrd = G[f"lr{dirn}"]
            evac(lrd, lrd[:], bank, bank[0:16, 2:258])
            bank = PR.next()
            p.op("tensor", lambda e, bank=bank, lrd=lrd: e.matmul(bank[0:64, 0:256], lhsT=up_sb[:, 64 * dirn:64 * dirn + 64], rhs=lrd[:], start=True, stop=True), reads=[up_sb, lrd], writes=[bank])
            gb = prm_sb[0:64, 40 + dirn:41 + dirn]
            p.op("vector", lambda e, bank=bank: e.tensor_scalar(out=G["zb"][:], in0=bank[0:64, 0:256], scalar1=gb, scalar2=None, op0=ALU.add), reads=[bank, prm_sb], writes=[G["zb"]])
            p.op("scalar", lambda e: e.activation(out=G["ta"][:], in_=G["zb"][:], func=AF.Abs), reads=[G["zb"]], writes=[G["ta"]])
            p.op("scalar", lambda e: e.activation(out=G["ex"][:], in_=G["ta"][:], func=AF.Exp, scale=-1.0), reads=[G["ta"]], writes=[G["ex"]])
            p.op("scalar", lambda e: e.activation(out=G["ta"][:], in_=G["ex"][:], func=AF.Ln, bias=1.0), reads=[G["ex"]], writes=[G["ta"]])
            p.op("vector", lambda e: e.scalar_tensor_tensor(out=G["gkk"][:], in0=G["zb"][:], scalar=0.0, in1=G["ta"][:], op0=ALU.min, op1=ALU.subtract), reads=[G["zb"], G["ta"]], writes=[G["gkk"]])
            p.op("vector", lambda e: e.tensor_scalar(out=G["gkk"][:], in0=G["gkk"][:], scalar1=1.0 / 16.0, scalar2=None, op0=ALU.mult), reads=[G["gkk"]], writes=[G["gkk"]])
            for tt in (0, 1):
                sl = slice(128 * tt, 128 * tt + 128)
                p.op("vector", lambda e, sl=sl: e.tensor_tensor_scan(out=G["gc"][:, sl], data0=G["ones"][:, sl], data1=G["gkk"][:, sl], initial=0.0, op0=ALU.mult, op1=ALU.add),
                     reads=[G["ones"], G["gkk"]], writes=[G["gc"]])
                p.op("vector", lambda e, tt=tt: e.tensor_copy(out=G["glc"][:, tt:tt + 1], in_=G["gc"][:, 128 * tt + 127:128 * tt + 128]), reads=[G["gc"]], writes=[G["glc"]])
                if dirn == 1:
                    p.op("vector", lambda e, sl=sl: e.scalar_tensor_tensor(out=G["gc"][:, sl], in0=G["gc"][:, sl], scalar=-1.0, in1=G["gkk"][:, sl], op0=ALU.mult, op1=ALU.add),
                         reads=[G["gc"], G["gkk"]], writes=[G["gc"]])
                    p.op("vector", lambda e, sl=sl, tt=tt: e.tensor_scalar(out=G["gc"][:, sl], in0=G["gc"][:, sl], scalar1=G["glc"][:, tt:tt + 1], scalar2=None, op0=ALU.add),
                         reads=[G["gc"], G["glc"]], writes=[G["gc"]])
            p.op("scalar", lambda e: e.activation(out=G["egl"][:], in_=G["glc"][:], func=AF.Exp), reads=[G["glc"]], writes=[G["egl"]])
            p.op("scalar", lambda e: e.activation(out=G["ex"][:], in_=G["gc"][:], func=AF.Exp), reads=[G["gc"]], writes=[G["ex"]])
            p.op("vector", lambda e: e.scalar_tensor_tensor(out=G["qpos"][:], in0=G["q"][:], scalar=64.0 ** -0.5, in1=G["ex"][:], op0=ALU.mult, op1=ALU.mult), reads=[G["q"], G["ex"]], writes=[G["qpos"]])
            p.op("scalar", lambda e: e.activation(out=G["ex"][:], in_=G["gc"][:], func=AF.Exp, scale=-1.0), reads=[G["gc"], G["qpos"]], writes=[G["ex"]])
            p.op("vector", lambda e: e.tensor_tensor(out=G["kneg"][:], in0=G["k"][:], in1=G["ex"][:], op=ALU.mult), reads=[G["k"], G["ex"]], writes=[G["kneg"]])
            for tt in (0, 1):
                sl = slice(128 * tt, 128 * tt + 128)
                p.op("scalar", lambda e, sl=sl, tt=tt: e.activation(out=G["ta"][:, sl], in_=G["gc"][:, sl], func=AF.Exp, scale=-1.0, bias=G["glc"][:, tt:tt + 1]),
                     reads=[G["gc"], G["glc"]], writes=[G["ta"]])
            p.op("vector", lambda e: e.tensor_tensor(out=G["ktl"][:], in0=G["k"][:], in1=G["ta"][:], op=ALU.mult), reads=[G["k"], G["ta"]], writes=[G["ktl"]])
            for tt in (0, 1):
                sl = slice(128 * tt, 128 * tt + 128)
                gt = GT[tt]
                q = QR.next()
                p.op("tensor", lambda e, q=q, sl=sl: e.matmul(q[:, 0:128], lhsT=G["kneg"][:, sl], rhs=G["qpos"][:, sl], start=True, stop=True), reads=[G["kneg"], G["qpos"]], writes=[q])
                p.op("vector", lambda e, q=q, gt=gt: e.tensor_tensor(out=gt["AT"][:], in0=q[:, 0:128], in1=GM, op=ALU.mult), reads=[q, cst_sb], writes=[gt["AT"]])
                q2 = QR.next()
                p.op("tensor", lambda e, q2=q2, sl=sl: e.matmul(q2[:, 0:64], lhsT=G["ktl"][:, sl], rhs=idb[0:64, 0:64], start=True, stop=True), reads=[G["ktl"], idb], writes=[q2])
                evac(gt["kt"], gt["kt"][:], q2, q2[:, 0:64])
            for tt in tiles:
                t0 = b * 256 + tt * 128
                sc = SC[tt]
                if dirn == 1:
                    p.dma("sync", sc["ofl"][:], ofw_t[t0 // 128][:, :], reads=[ofw_t[t0 // 128]], writes=[sc["ofl"]], sembuf=sc["ofl"])
                for ch in [c for c in chs if c["tt"] == tt]:
                    s = ch["s"]; dvw = ch["dvw"]; si = 0 if s["name"] == "A" else 1
                    q = QR.next()
                    p.op("tensor", lambda e, q=q, ch=ch, s=s, dvw=dvw: e.matmul(q[:, 0:dvw], lhsT=ch["wT"][:], rhs=s["Sb"][:], start=True, stop=True), reads=[ch["wT"], s["Sb"]], writes=[q])
                    p.op("vector", lambda e, q=q, ch=ch, dvw=dvw: e.tensor_tensor(out=ch["vn"][:], in0=ch["u"][:], in1=q[:, 0:dvw], op=ALU.subtract), reads=[ch["u"], q], writes=[ch["vn"]])
                    qo = QR.next()
                    p.op("tensor", lambda e, qo=qo, ch=ch, s=s, dvw=dvw: e.matmul(qo[:, 0:dvw], lhsT=ch["qdT"][:], rhs=s["Sb"][:], start=True, stop=False), reads=[ch["qdT"], s["Sb"]], writes=[qo])
                    p.op("tensor", lambda e, qo=qo, ch=ch, dvw=dvw: e.matmul(qo[:, 0:dvw], lhsT=ch["qkDT"][:], rhs=ch["vn"][:], start=False, stop=True), reads=[ch["qkDT"], ch["vn"]], writes=[qo])
                    qs = QR.next()
                    p.op("tensor", lambda e, qs=qs, ch=ch, dvw=dvw: e.matmul(qs[:, 0:dvw], lhsT=ch["kt"][:], rhs=ch["vn"][:], start=True, stop=True), reads=[ch["kt"], ch["vn"]], writes=[qs])
                    p.op("vector", lambda e, qs=qs, s=s, sc=sc, si=si, dvw=dvw: e.scalar_tensor_tensor(out=s["S"][:], in0=s["S"][:], scalar=sc["egl"][:, si:si + 1], in1=qs[:, 0:dvw], op0=ALU.mult, op1=ALU.add),
                         reads=[s["S"], sc["egl"], qs], writes=[s["S"]])
                    p.op("scalar", lambda e, s=s: e.copy(out=s["Sb"][:], in_=s["S"][:]), reads=[s["S"]], writes=[s["Sb"]])
                    oc = s["oc"]
                    if dirn == 0:
                        p.op("scalar", lambda e, ch=ch, qo=qo, dvw=dvw: e.copy(out=ch["osb"][:], in_=qo[:, 0:dvw]), reads=[qo], writes=[ch["osb"]])
                        p.dma("sync", ofw_t[t0 // 128][:, oc:oc + dvw], ch["osb"][:], reads=[ch["osb"]], writes=[ofw_t[t0 // 128]], sembuf=ch["osb"])
                    else:
                        p.op("vector", lambda e, ch=ch, qo=qo, dvw=dvw, sc=sc, oc=oc: e.tensor_tensor(out=ch["osb"][:], in0=qo[:, 0:dvw], in1=sc["ofl"][:, oc:oc + dvw], op=ALU.add), reads=[qo, sc["ofl"]], writes=[ch["osb"]])
                        dst = oA if s["name"] == "A" else oB
                        p.dma("sync", dst[t0:t0 + 128, :], ch["osb"][:], reads=[ch["osb"]], sembuf=ch["osb"])
                gt = GT[tt]; sl = slice(128 * tt, 128 * tt + 128)
                qo = QR.next()
                p.op("tensor", lambda e, qo=qo, sl=sl: e.matmul(qo[:, 0:128], lhsT=G["qpos"][:, sl], rhs=G["Sb"][:], start=True, stop=False), reads=[G["qpos"], G["Sb"]], writes=[qo])
                p.op("tensor", lambda e, qo=qo, gt=gt, sc=sc: e.matmul(qo[:, 0:128], lhsT=gt["AT"][:], rhs=sc["gvb"][:], start=False, stop=True), reads=[gt["AT"], sc["gvb"]], writes=[qo])
                qs = QR.next()
                p.op("tensor", lambda e, qs=qs, gt=gt, sc=sc: e.matmul(qs[0:64, 0:128], lhsT=gt["kt"][:], rhs=sc["gvb"][:], start=True, stop=True), reads=[gt["kt"], sc["gvb"]], writes=[qs])
                p.op("vector", lambda e, qs=qs, tt=tt: e.scalar_tensor_tensor(out=G["S"][:], in0=G["S"][:], scalar=G["egl"][:, tt:tt + 1], in1=qs[0:64, 0:128], op0=ALU.mult, op1=ALU.add),
                     reads=[G["S"], G["egl"], qs], writes=[G["S"]])
                p.op("scalar", lambda e: e.copy(out=G["Sb"][:], in_=G["S"][:]), reads=[G["S"]], writes=[G["Sb"]])
                if dirn == 0:
                    p.op("scalar", lambda e, gt=gt, qo=qo: e.copy(out=gt["osb"][:], in_=qo[:, 0:128]), reads=[qo], writes=[gt["osb"]])
                    p.dma("sync", ofw_t[t0 // 128][:, 192:320], gt["osb"][:], reads=[gt["osb"]], writes=[ofw_t[t0 // 128]], sembuf=gt["osb"])
                else:
                    p.op("vector", lambda e, gt=gt, qo=qo, sc=sc: e.tensor_tensor(out=gt["osb"][:], in0=qo[:, 0:128], in1=sc["ofl"][:, 192:320], op=ALU.add), reads=[qo, sc["ofl"]], writes=[gt["osb"]])
                    p.dma("sync", ogl[t0:t0 + 128, :], gt["osb"][:], reads=[gt["osb"]], sembuf=gt["osb"])


def build_att(S, xdt=BF16):
    p = PB(); nc = p.nc
    NT = S // 128; NB = S // 256; NQB = NB // 2; SQ = S // 2
    ext = lambda n, s, d: nc.dram_tensor(n, s, d, kind="ExternalInput").ap()
    xb = ext("xb", [NB, 128, 16 * 260], xdt)
    xq = ext("xq", [NQB, 128, 16 * 260], xdt)
    wfa = ext("wfa", [128, 16 * 512], F32)
    wta = ext("wta", [128, 16 * 128], F32)
    prm = ext("prm", [128, 64], F32)
    cst = ext("cst", [128, len(CN) * 128], F32)
    cosk = ext("cosk", [128, S], F32); sink = ext("sink", [128, S], F32)
    cosq = ext("cosq", [128, SQ], F32); sinq = ext("sinq", [128, SQ], F32)
    oat = nc.dram_tensor("oat", [SQ, 384], F32, kind="ExternalOutput").ap()
    phase_att(p, S, xdt, None, xb, xq, wfa, wta, prm, cst, cosk, sink, cosq, sinq, oat)
    p.finish()
    print("att instr", p.ninstr, "sems", len(p.sems))
    return p.emit()


def phase_att(p, S, xdt, banks, xb, xq, wfa, wta, prm, cst, cosk, sink, cosq, sinq, oat):
    nc = p.nc
    NT = S // 128; NB = S // 256; NQB = NB // 2; SQ = S // 2

    wfa_sb = p.sb("wfa_sb", [128, 16, 512], BF16)
    wta_sb = p.sb("wta_sb", [128, 16, 128], BF16)
    prm_sb = p.sb("prm_sb_a", [128, 64], F32)
    cst_sb = p.sb("cst_sb_a", [128, len(CN) * 128], F32)
    C = {n: cst_sb[:, i * 128:(i + 1) * 128] for i, n in enumerate(CN)}
    onesb = p.sb("onesb", [128, 128], BF16)
    for k in range(16):
        p.dma("gpsimd", wfa_sb[:, k, :], wfa[:, k * 512:(k + 1) * 512], writes=[wfa_sb])
        p.dma("gpsimd", wta_sb[:, k, :], wta[:, k * 128:(k + 1) * 128], writes=[wta_sb])
    p.dma("sync", prm_sb[:], prm[:, :], writes=[prm_sb])
    p.dma("sync", cst_sb[:], cst[:, :], writes=[cst_sb])
    p.op("vector", lambda e: e.tensor_copy(out=onesb[:], in_=C["ONES"]), reads=[cst_sb], writes=[onesb])

    KT = p.sb("KT", [128, S], BF16)
    V = p.sb("V", [128, NT, 132], BF16)
    QT = [p.sb(f"QT{h}", [128, SQ], BF16) for h in range(3)]
    p.op("gpsimd", lambda e: e.memset(V[:, :, 128:129], 1.0), writes=[V])
    xblk = [p.sb(f"xblka{i}", [128, 16, 260], BF16) for i in range(2)]
    cosb = [p.sb(f"cosb{i}", [128, 256], F32) for i in range(2)]
    sinb = [p.sb(f"sinb{i}", [128, 256], F32) for i in range(2)]
    W = {}
    for i in range(2):
        for nm, dt in [("sq", BF16), ("rs", F32), ("kn", F32), ("t1", F32), ("t2", F32)]:
            W[nm, i] = p.sb(f"w_{nm}{i}", [128, 256], dt)
    if banks is None:
        banks = [p.ps(f"bank{i}", [128, 512], F32) for i in range(8)]
    PR = Ring(banks[0:4])
    wi = [0]

    def normrope(bank, gcol, cb, sb_, dst_buf, dst_ap):
        i = wi[0] % 2; wi[0] += 1
        sq, rs, kn, t1, t2 = (W[n, i] for n in ("sq", "rs", "kn", "t1", "t2"))
        raw = bank[:, 2:258]
        p.op("scalar", lambda e: e.activation(out=sq[:], in_=raw, func=AF.Square), reads=[bank], writes=[sq])
        b2 = PR.next()
        p.op("tensor", lambda e: e.matmul(b2[:, 0:256], lhsT=onesb[:], rhs=sq[:], start=True, stop=True), reads=[onesb, sq], writes=[b2])
        p.op("scalar", lambda e: e.activation(out=rs[:], in_=b2[:, 0:256], func=AF.Sqrt, scale=1.0 / 128.0, bias=EPS), reads=[b2], writes=[rs])
        p.op("vector", lambda e: e.reciprocal(out=rs[:], in_=rs[:]), reads=[rs], writes=[rs])
        p.op("vector", lambda e: e.scalar_tensor_tensor(out=kn[:], in0=raw, scalar=gcol, in1=rs[:], op0=ALU.mult, op1=ALU.mult), reads=[bank, prm_sb, rs], writes=[kn])
        b3 = PR.next()
        p.op("tensor", lambda e: e.matmul(b3[:, 0:256], lhsT=C["ROT"], rhs=kn[:], start=True, stop=True), reads=[cst_sb, kn], writes=[b3])
        p.op("gpsimd", lambda e: e.tensor_tensor(out=t1[:], in0=kn[:], in1=cb[:], op=ALU.mult), reads=[kn, cb], writes=[t1])
        p.op("vector", lambda e: e.tensor_tensor(out=t2[:], in0=b3[:, 0:256], in1=sb_[:], op=ALU.mult), reads=[b3, sb_], writes=[t2])
        p.op("vector", lambda e: e.tensor_tensor(out=dst_ap, in0=t1[:], in1=t2[:], op=ALU.add), reads=[t1, t2], writes=[dst_buf])

    def fm_proj(blk, m0):
        bank = PR.next()
        for k in range(16):
            p.op("tensor", lambda e, k=k: e.matmul(bank[:, 0:260], lhsT=wfa_sb[:, k, m0:m0 + 128], rhs=blk[:, k, :], start=(k == 0), stop=(k == 15)),
                 reads=[wfa_sb, blk], writes=[bank], inc=(k == 15))
        return bank

    def load_blk(i, src, b, ctab, stab):
        blk = xblk[i % 2]
        if xdt == BF16:
            p.dma("sync", blk[:].rearrange("p k t -> p (k t)"), src[b, :, :], writes=[blk])
        else:
            for k in range(16):
                p.dma("gpsimd", blk[:, k, :], src[b, :, k * 260:(k + 1) * 260], writes=[blk])
        cb = cosb[i % 2]; sb_ = sinb[i % 2]
        p.dma("sync", cb[:], ctab[:, b * 256:(b + 1) * 256], writes=[cb])
        p.dma("sync", sb_[:], stab[:, b * 256:(b + 1) * 256], writes=[sb_])
        return blk, cb, sb_

    for b in range(NB):
        blk, cb, sb_ = load_blk(b, xb, b, cosk, sink)
        bank = fm_proj(blk, 384)
        normrope(bank, prm_sb[:, 39:40], cb, sb_, KT, KT[:, b * 256:(b + 1) * 256])
        for tt in range(2):
            bank = PR.next()
            for k in range(16):
                p.op("tensor", lambda e, k=k, tt=tt, bank=bank: e.matmul(bank[:, 0:128], lhsT=blk[:, k, 2 + 128 * tt:2 + 128 * (tt + 1)], rhs=wta_sb[:, k, :], start=(k == 0), stop=(k == 15)),
                     reads=[blk, wta_sb], writes=[bank], inc=(k == 15))
            p.op("scalar", lambda e, bank=bank, tt=tt: e.copy(out=V[:, 2 * b + tt, 0:128], in_=bank[:, 0:128]), reads=[bank], writes=[V])
    for b in range(NQB):
        blk, cb, sb_ = load_blk(NB + b, xq, b, cosq, sinq)
        for h in range(3):
            bank = fm_proj(blk, 128 * h)
            normrope(bank, prm_sb[:, 38:39], cb, sb_, QT[h], QT[h][:, b * 256:(b + 1) * 256])
    pT = [p.sb(f"pT{i}", [128, 512], BF16) for i in range(3)]
    rec = [p.sb(f"rec{i}", [128, 1], F32) for i in range(4)]
    osb = [p.sb(f"osba{i}", [128, 128], F32) for i in range(4)]
    PSR = Ring(banks[0:4])
    po = banks[4:8]
    scale = 128.0 ** -0.5
    it = 0
    QW = min(512, SQ)
    NQS = QW // 128
    for h in range(3):
        for qb in range(SQ // QW):
            for kt in range(NT):
                ps = PSR.next()
                p.op("tensor", lambda e, ps=ps, kt=kt, h=h, qb=qb: e.matmul(ps[:, 0:QW], lhsT=KT[:, kt * 128:(kt + 1) * 128], rhs=QT[h][:, qb * QW:(qb + 1) * QW], start=True, stop=True),
                     reads=[KT, QT[h]], writes=[ps])
                pt = pT[it % 3]; it += 1
                p.op("scalar", lambda e, ps=ps, pt=pt: e.activation(out=pt[:, 0:QW], in_=ps[:, 0:QW], func=AF.Exp, scale=scale), reads=[ps], writes=[pt])
                for qs in range(NQS):
                    p.op("tensor", lambda e, qs=qs, pt=pt, kt=kt: e.matmul(po[qs][:, 0:129], lhsT=pt[:, qs * 128:(qs + 1) * 128], rhs=V[:, kt, 0:129], start=(kt == 0), stop=(kt == NT - 1)),
                         reads=[pt, V], writes=[po[qs]], inc=(kt == NT - 1))
            for qs in range(NQS):
                p.op("vector", lambda e, qs=qs: e.reciprocal(out=rec[qs][:], in_=po[qs][:, 128:129]), reads=[po[qs]], writes=[rec[qs]])
                p.op("scalar", lambda e, qs=qs: e.activation(out=osb[qs][:], in_=po[qs][:, 0:128], func=AF.Copy, scale=rec[qs][:]), reads=[po[qs], rec[qs]], writes=[osb[qs]])
                q0 = qb * QW + qs * 128
                p.dma("sync", oat[q0:q0 + 128, h * 128:(h + 1) * 128], osb[qs][:], reads=[osb[qs]], sembuf=osb[qs])


ALPHA = (2.0 * 4) ** 0.25


def layernorm_tile(p, z, out, lng, lnb, st, mv, tmp):
    for c in range(4):
        p.op("vector", lambda e, c=c: e.bn_stats(out=st[:, c, :], in_=z[:, c * 512:(c + 1) * 512]), reads=[z], writes=[st])
    p.op("vector", lambda e: e.bn_aggr(out=mv[:, 0:2], in_=st[:].rearrange("p a b -> p (a b)")), reads=[st], writes=[mv])
    p.op("scalar", lambda e: e.activation(out=mv[:, 2:3], in_=mv[:, 1:2], func=AF.Sqrt, bias=EPS, scale=1.0), reads=[mv], writes=[mv])
    p.op("vector", lambda e: e.reciprocal(out=mv[:, 3:4], in_=mv[:, 2:3]), reads=[mv], writes=[mv])
    p.op("vector", lambda e: e.tensor_scalar(out=tmp[:], in0=z[:], scalar1=mv[:, 0:1], scalar2=mv[:, 3:4], op0=ALU.subtract, op1=ALU.mult), reads=[z, mv], writes=[tmp])
    p.op("gpsimd", lambda e: e.tensor_tensor(out=tmp[:], in0=tmp[:], in1=lng[:], op=ALU.mult), reads=[tmp, lng], writes=[tmp])
    p.op("vector", lambda e: e.tensor_tensor(out=out[:], in0=tmp[:], in1=lnb[:], op=ALU.add), reads=[tmp, lnb], writes=[out])


def transpose_out(p, src_f32, banks4, idf, xTf, xTb):
    for g in range(4):
        bank = banks4[g]
        for j in range(4):
            c = 4 * g + j
            p.op("tensor", lambda e, c=c, j=j, bank=bank: e.matmul(bank[:, j * 128:(j + 1) * 128], lhsT=src_f32[:, c * 128:(c + 1) * 128], rhs=idf[:], start=True, stop=True),
                 reads=[src_f32, idf], writes=[bank])
        p.op("scalar", lambda e, g=g, bank=bank: e.copy(out=xTf[:, 4 * g:4 * g + 4, :].rearrange("p a b -> p (a b)"), in_=bank[:, :]), reads=[bank], writes=[xTf])
    p.op("gpsimd", lambda e: e.tensor_copy(out=xTb[:], in_=xTf[:]), reads=[xTf], writes=[xTb])


def build_post(TT, moe):
    p = PB(); nc = p.nc
    T = TT * 128
    ext = lambda n, s, d: nc.dram_tensor(n, s, d, kind="ExternalInput").ap()
    out = lambda n, s, d: nc.dram_tensor(n, s, d, kind="ExternalOutput").ap()
    o_in = ext("o", [T, 2048], F32); gt_in = ext("gt", [T, 1280], F32); x_in = ext("x", [T, 2048], F32)
    wout = ext("wout", [128, 16 * 2048], F32)
    nrm = ext("nrm", [128, 256], F32)
    ln = ext("ln", [128, 4096], F32)
    ident = ext("ident", [128, 128], F32)
    x1_out = out("x1", [T, 2048], F32); x1T_out = out("x1T", [TT, 128, 16 * 128], BF16)
    if moe:
        rw = ext("rw", [128, 16 * 8], F32)
        g_out = out("gates", [T, 8], F32)
    wout_sb = p.sb("wout_sb", [128, 16, 2048], BF16)
    for k in range(16):
        p.dma("gpsimd", wout_sb[:, k, :], wout[:, k * 2048:(k + 1) * 2048], writes=[wout_sb])
    nrm_sb = p.sb("nrm_sb", [128, 256], F32); lng = p.sb("lng", [128, 2048], F32); lnb = p.sb("lnb", [128, 2048], F32)
    idf = p.sb("idf", [128, 128], F32); idb = p.sb("idb", [128, 128], BF16)
    p.dma("sync", nrm_sb[:], nrm[:, :], writes=[nrm_sb])
    p.dma("sync", lng[:], ln[:, 0:2048], writes=[lng]); p.dma("sync", lnb[:], ln[:, 2048:4096], writes=[lnb])
    p.dma("sync", idf[:], ident[:, :], writes=[idf])
    p.op("vector", lambda e: e.tensor_copy(out=idb[:], in_=idf[:]), reads=[idf], writes=[idb])
    if moe:
        rw_sb = p.sb("rw_sb", [128, 16, 8], F32)
        p.dma("sync", rw_sb[:].rearrange("p a b -> p (a b)"), rw[:, :], writes=[rw_sb])
    banks = [p.ps(f"bank{i}", [128, 512], F32) for i in range(8)]
    ob = [p.sb(f"ob{i}", [128, 2048], F32) for i in range(2)]
    gb = [p.sb(f"gb{i}", [128, 1280], F32) for i in range(2)]
    xbf = [p.sb(f"xb{i}", [128, 2048], F32) for i in range(2)]
    sq = p.sb("sq", [128, 1280], F32); ss = p.sb("ss", [128, 32], F32); sg = p.sb("sg", [128, 1280], F32)
    mix = p.sb("mix", [128, 2048], BF16); mixT = p.sb("mixT", [128, 16, 128], BF16)
    z = p.sb("z", [128, 2048], F32); x1 = p.sb("x1s", [128, 2048], F32); tmp = p.sb("tmp", [128, 2048], F32)
    st = p.sb("st", [128, 4, 6], F32); mv = p.sb("mv", [128, 4], F32)
    xTf = p.sb("xTf", [128, 16, 128], F32); xTb = p.sb("xTb", [128, 16, 128], BF16)
    if moe:
        lg = p.sb("lg", [128, 8], F32); mx = p.sb("mx", [128, 8], F32); gs = p.sb("gs", [128, 8], F32); ga = p.sb("ga", [128, 8], F32); gbb = p.sb("gbb", [128, 8], F32)
    for t in range(TT):
        o = ob[t % 2]; g = gb[t % 2]; x = xbf[t % 2]
        r0 = t * 128
        p.dma("sync", o[:], o_in[r0:r0 + 128, :], writes=[o])
        p.dma("sync", g[:], gt_in[r0:r0 + 128, :], writes=[g])
        p.dma("sync", x[:], x_in[r0:r0 + 128, :], writes=[x])
        p.op("vector", lambda e, o=o: e.tensor_tensor(out=sq[:, 0:768], in0=o[:, 0:768], in1=o[:, 0:768], op=ALU.mult), reads=[o], writes=[sq])
        p.op("gpsimd", lambda e, o=o: e.tensor_tensor(out=sq[:, 768:1280], in0=o[:, 1536:2048], in1=o[:, 1536:2048], op=ALU.mult), reads=[o], writes=[sq])
        p.op("vector", lambda e: e.tensor_reduce(out=ss[:, 0:10], in_=sq[:].rearrange("p (h d) -> p h d", d=128), axis=AX.X, op=ALU.add), reads=[sq], writes=[ss])
        p.op("scalar", lambda e: e.activation(out=ss[:, 10:20], in_=ss[:, 0:10], func=AF.Sqrt, scale=1.0 / 128.0, bias=EPS), reads=[ss], writes=[ss])
        p.op("vector", lambda e: e.reciprocal(out=ss[:, 20:30], in_=ss[:, 10:20]), reads=[ss], writes=[ss])
        p.op("scalar", lambda e, g=g: e.activation(out=sg[:], in_=g[:], func=AF.Silu), reads=[g], writes=[sg])
        for h in range(10):
            oc = h * 128 if h < 6 else 1536 + (h - 6) * 128
            gcol = nrm_sb[:, 0:128] if h < 6 else nrm_sb[:, 128:256]
            p.op("vector", lambda e, h=h, oc=oc, gcol=gcol, o=o: e.scalar_tensor_tensor(out=sq[:, h * 128:(h + 1) * 128], in0=o[:, oc:oc + 128], scalar=ss[:, 20 + h:21 + h], in1=gcol, op0=ALU.mult, op1=ALU.mult),
                 reads=[o, ss, nrm_sb, sq], writes=[sq])
        p.op("vector", lambda e: e.tensor_tensor(out=mix[:, 0:768], in0=sq[:, 0:768], in1=sg[:, 0:768], op=ALU.mult), reads=[sq, sg], writes=[mix])
        p.op("gpsimd", lambda e: e.tensor_tensor(out=mix[:, 1536:2048], in0=sq[:, 768:1280], in1=sg[:, 768:1280], op=ALU.mult), reads=[sq, sg, mix], writes=[mix])
        p.op("gpsimd", lambda e, o=o: e.tensor_copy(out=mix[:, 768:1536], in_=o[:, 768:1536]), reads=[o, mix], writes=[mix])
        for gq in range(4):
            bank = banks[gq]
            for j in range(4):
                c = 4 * gq + j
                p.op("tensor", lambda e, c=c, j=j, bank=bank: e.matmul(bank[:, j * 128:(j + 1) * 128], lhsT=mix[:, c * 128:(c + 1) * 128], rhs=idb[:], start=True, stop=True),
                     reads=[mix, idb], writes=[bank])
            p.op("scalar", lambda e, gq=gq, bank=bank: e.copy(out=mixT[:, 4 * gq:4 * gq + 4, :].rearrange("p a b -> p (a b)"), in_=bank[:, :]), reads=[bank], writes=[mixT])
        for nb in range(4):
            bank = banks[4 + nb]
            for k in range(16):
                p.op("tensor", lambda e, k=k, nb=nb, bank=bank: e.matmul(bank[:, :], lhsT=mixT[:, k, :], rhs=wout_sb[:, k, nb * 512:(nb + 1) * 512], start=(k == 0), stop=(k == 15)),
                     reads=[mixT, wout_sb], writes=[bank], inc=(k == 15))
            p.op("vector", lambda e, nb=nb, bank=bank, x=x: e.scalar_tensor_tensor(out=z[:, nb * 512:(nb + 1) * 512], in0=x[:, nb * 512:(nb + 1) * 512], scalar=ALPHA, in1=bank[:, :], op0=ALU.mult, op1=ALU.add),
                 reads=[x, bank, z], writes=[z])
        layernorm_tile(p, z, x1, lng, lnb, st, mv, tmp)
        p.dma("sync", x1_out[r0:r0 + 128, :], x1[:], reads=[x1], sembuf=x1)
        transpose_out(p, x1, banks[0:4], idf, xTf, xTb)
        p.dma("sync", x1T_out[t, :, :], xTb[:].rearrange("p a b -> p (a b)"), reads=[xTb], sembuf=xTb)
        if moe:
            bank = banks[4]
            for k in range(16):
                p.op("tensor", lambda e, k=k: e.matmul(bank[:, 0:8], lhsT=xTf[:, k, :], rhs=rw_sb[:, k, :], start=(k == 0), stop=(k == 15)), reads=[xTf, rw_sb], writes=[bank], inc=(k == 15))
            p.op("scalar", lambda e: e.copy(out=lg[:], in_=bank[:, 0:8]), reads=[bank], writes=[lg])
            p.op("vector", lambda e: e.max(out=mx[:], in_=lg[:]), reads=[lg], writes=[mx])
            p.op("vector", lambda e: e.tensor_tensor(out=gs[:, 0:1], in0=mx[:, 1:2], in1=mx[:, 0:1], op=ALU.subtract), reads=[mx], writes=[gs])
            p.op("scalar", lambda e: e.activation(out=gs[:, 1:2], in_=gs[:, 0:1], func=AF.Exp), reads=[gs], writes=[gs])
            p.op("vector", lambda e: e.tensor_scalar(out=gs[:, 2:3], in0=gs[:, 1:2], scalar1=1.0, scalar2=None, op0=ALU.add), reads=[gs], writes=[gs])
            p.op("vector", lambda e: e.reciprocal(out=gs[:, 3:4], in_=gs[:, 2:3]), reads=[gs], writes=[gs])
            p.op("vector", lambda e: e.tensor_tensor(out=gs[:, 4:5], in0=gs[:, 1:2], in1=gs[:, 3:4], op=ALU.mult), reads=[gs], writes=[gs])
            p.op("vector", lambda e: e.tensor_scalar(out=ga[:], in0=lg[:], scalar1=mx[:, 0:1], scalar2=gs[:, 3:4], op0=ALU.is_equal, op1=ALU.mult), reads=[lg, mx, gs], writes=[ga])
            p.op("vector", lambda e: e.tensor_scalar(out=gbb[:], in0=lg[:], scalar1=mx[:, 1:2], scalar2=gs[:, 4:5], op0=ALU.is_equal, op1=ALU.mult), reads=[lg, mx, gs], writes=[gbb])
            p.op("vector", lambda e: e.tensor_tensor(out=ga[:], in0=ga[:], in1=gbb[:], op=ALU.add), reads=[ga, gbb], writes=[ga])
            p.dma("sync", g_out[r0:r0 + 128, :], ga[:], reads=[ga], sembuf=ga)
    p.finish()
    print("post instr", p.ninstr, "sems", len(p.sems))
    return p.emit()


def build_combine(TT, moe, NE=8):
    p = PB(); nc = p.nc
    T = TT * 128
    ext = lambda n, s, d: nc.dram_tensor(n, s, d, kind="ExternalInput").ap()
    out = lambda n, s, d: nc.dram_tensor(n, s, d, kind="ExternalOutput").ap()
    x1_in = ext("x1", [T, 2048], F32); y_in = ext("y", [NE, T, 2048], BF16)
    ln = ext("ln", [128, 4096], F32); ident = ext("ident", [128, 128], F32)
    if moe:
        g_in = ext("gates", [T, 8], F32)
    x2_out = out("x2", [T, 2048], F32); x2T_out = out("x2T", [TT, 128, 16 * 128], BF16)
    lng = p.sb("lng", [128, 2048], F32); lnb = p.sb("lnb", [128, 2048], F32); idf = p.sb("idf", [128, 128], F32)
    p.dma("sync", lng[:], ln[:, 0:2048], writes=[lng]); p.dma("sync", lnb[:], ln[:, 2048:4096], writes=[lnb]); p.dma("sync", idf[:], ident[:, :], writes=[idf])
    banks = [p.ps(f"bank{i}", [128, 512], F32) for i in range(4)]
    xb_ = [p.sb(f"x1b{i}", [128, 2048], F32) for i in range(2)]
    yb = [p.sb(f"yb{i}", [128, 2048], BF16) for i in range(3)]
    gsb = [p.sb(f"gsb{i}", [128, 8], F32) for i in range(2)]
    acc = p.sb("acc", [128, 2048], F32); x2 = p.sb("x2s", [128, 2048], F32); tmp = p.sb("tmp", [128, 2048], F32)
    st = p.sb("st", [128, 4, 6], F32); mv = p.sb("mv", [128, 4], F32)
    xTf = p.sb("xTf", [128, 16, 128], F32); xTb = p.sb("xTb", [128, 16, 128], BF16)
    yi = 0
    for t in range(TT):
        r0 = t * 128
        x1 = xb_[t % 2]
        p.dma("sync", x1[:], x1_in[r0:r0 + 128, :], writes=[x1])
        if moe:
            gt = gsb[t % 2]
            p.dma("sync", gt[:], g_in[r0:r0 + 128, :], writes=[gt])
        p.op("vector", lambda e, x1=x1: e.tensor_scalar(out=acc[:], in0=x1[:], scalar1=ALPHA, scalar2=None, op0=ALU.mult), reads=[x1, acc], writes=[acc])
        for ei in range(NE):
            y = yb[yi % 3]; yi += 1
            p.dma("sync" if ei % 2 == 0 else "scalar", y[:], y_in[ei, r0:r0 + 128, :], writes=[y])
            if moe:
                p.op("vector", lambda e, y=y, ei=ei, gt=gt: e.scalar_tensor_tensor(out=acc[:], in0=y[:], scalar=gt[:, ei:ei + 1], in1=acc[:], op0=ALU.mult, op1=ALU.add), reads=[y, gt, acc], writes=[acc])
            else:
                p.op("vector", lambda e, y=y: e.tensor_tensor(out=acc[:], in0=y[:], in1=acc[:], op=ALU.add), reads=[y, acc], writes=[acc])
        layernorm_tile(p, acc, x2, lng, lnb, st, mv, tmp)
        p.dma("sync", x2_out[r0:r0 + 128, :], x2[:], reads=[x2], sembuf=x2)
        transpose_out(p, x2, banks, idf, xTf, xTb)
        p.dma("sync", x2T_out[t, :, :], xTb[:].rearrange("p a b -> p (a b)"), reads=[xTb], sembuf=xTb)
    p.finish()
    print("combine instr", p.ninstr, "sems", len(p.sems))
    return p.emit()


def build_ffn(NP, F, gc, TP=1024):
    p = PB(); nc = p.nc
    NCH = F // 128; NG = NCH // gc; GW = gc * 128
    NTB = TP // 512; NTT = TP // 128
    ext = lambda n, s, d: nc.dram_tensor(n, s, d, kind="ExternalInput").ap()
    xT = ext("xT", [NP, 128, 16 * TP], BF16)
    wg = ext("wg", [128, 16 * F], F32); wu = ext("wu", [128, 16 * F], F32); wd = ext("wd", [128, NCH * 2048], F32)
    y_out = nc.dram_tensor("y", [NP * TP, 2048], BF16, kind="ExternalOutput").ap()
    banks = [p.ps(f"bank{i}", [128, 512], F32) for i in range(8)]
    HB = Ring(banks[0:4]); YB = Ring(banks[4:8])
    xs = [p.sb(f"xs{i}", [128, 16, TP], BF16) for i in range(1)]
    wgs = [p.sb(f"wgs{i}", [128, 16, GW], BF16) for i in range(2)]
    wus = [p.sb(f"wus{i}", [128, 16, GW], BF16) for i in range(2)]
    wds = [p.sb(f"wds{i}", [128, gc, 2048], BF16) for i in range(2)]
    hT = [p.sb(f"hT{i}", [128, gc, TP], BF16) for i in range(2)]
    sgb = [p.sb(f"sgb{i}", [128, 512], F32) for i in range(2)]
    yacc = p.sb("yacc", [128, NTT, 2048], F32)
    yob = [p.sb(f"yob{i}", [128, 2048], BF16) for i in range(2)]
    gi = 0; si = 0
    for ps_ in range(NP):
        x = xs[0]
        p.dma("sync", x[:].rearrange("p k t -> p (k t)"), xT[ps_, :, :], writes=[x])
        for g in range(NG):
            wg_s = wgs[gi % 2]; wu_s = wus[gi % 2]; wd_s = wds[gi % 2]; h = hT[gi % 2]; gi += 1
            f0 = g * GW
            wg3 = wg.rearrange("p (k f) -> p k f", k=16); wu3 = wu.rearrange("p (k f) -> p k f", k=16)
            for kh in range(2):
                p.dma("gpsimd", wg_s[:, 8 * kh:8 * kh + 8, :], wg3[:, 8 * kh:8 * kh + 8, f0:f0 + GW], writes=[wg_s])
                p.dma("gpsimd", wu_s[:, 8 * kh:8 * kh + 8, :], wu3[:, 8 * kh:8 * kh + 8, f0:f0 + GW], writes=[wu_s])
            p.dma("gpsimd", wd_s[:].rearrange("p c d -> p (c d)"), wd[:, g * gc * 2048:(g + 1) * gc * 2048], writes=[wd_s], max_dma_last_dim=8192)
            for c in range(gc):
                for tb in range(NTB):
                    pg = HB.next(); pu = HB.next()
                    for k in range(16):
                        p.op("tensor", lambda e, k=k, c=c, tb=tb, pg=pg: e.matmul(pg[:, :], lhsT=wg_s[:, k, c * 128:(c + 1) * 128], rhs=x[:, k, tb * 512:(tb + 1) * 512], start=(k == 0), stop=(k == 15)), reads=[wg_s, x], writes=[pg], inc=(k == 15))
                    for k in range(16):
                        p.op("tensor", lambda e, k=k, c=c, tb=tb, pu=pu: e.matmul(pu[:, :], lhsT=wu_s[:, k, c * 128:(c + 1) * 128], rhs=x[:, k, tb * 512:(tb + 1) * 512], start=(k == 0), stop=(k == 15)), reads=[wu_s, x], writes=[pu], inc=(k == 15))
                    s_ = sgb[si % 2]; si += 1
                    p.op("scalar", lambda e, pg=pg, s_=s_: e.activation(out=s_[:], in_=pg[:, :], func=AF.Silu), reads=[pg], writes=[s_])
                    p.op("vector", lambda e, pu=pu, s_=s_, c=c, tb=tb, h=h: e.tensor_tensor(out=h[:, c, tb * 512:(tb + 1) * 512], in0=pu[:, :], in1=s_[:], op=ALU.mult), reads=[pu, s_, h], writes=[h])
            for t in range(NTT):
                for db in range(4):
                    py = YB.next()
                    for c in range(gc):
                        p.op("tensor", lambda e, c=c, t=t, db=db, py=py: e.matmul(py[:, :], lhsT=h[:, c, t * 128:(t + 1) * 128], rhs=wd_s[:, c, db * 512:(db + 1) * 512], start=(c == 0), stop=(c == gc - 1)), reads=[h, wd_s], writes=[py], inc=(c == gc - 1))
                    if g == 0:
                        p.op("scalar", lambda e, t=t, db=db, py=py: e.copy(out=yacc[:, t, db * 512:(db + 1) * 512], in_=py[:, :]), reads=[py, yacc], writes=[yacc])
                    else:
                        p.op("vector", lambda e, t=t, db=db, py=py: e.tensor_tensor(out=yacc[:, t, db * 512:(db + 1) * 512], in0=py[:, :], in1=yacc[:, t, db * 512:(db + 1) * 512], op=ALU.add), reads=[py, yacc], writes=[yacc])
        for t in range(NTT):
            r0 = ps_ * TP + t * 128
            yo = yob[t % 2]
            p.op("gpsimd", lambda e, t=t, yo=yo: e.tensor_copy(out=yo[:], in_=yacc[:, t, :]), reads=[yacc], writes=[yo])
            p.dma("sync", y_out[r0:r0 + 128, :], yo[:], reads=[yo], sembuf=yo)
    p.finish()
    print("ffn instr", p.ninstr, "sems", len(p.sems))
    return p.emit()


import numpy as np
OFF = np.cumsum([0, 768, 768, 768, 768, 6, 6, 6, 6, 768, 256, 256, 256, 256, 512, 512, 16, 16])
NAMES = ["dq", "dk", "dv", "dgate", "a_f", "a_b", "b_f", "b_b", "aq", "ak", "av", "gq", "gkk", "gv", "ggate", "lr_f", "lr_b"]
COL = {n: int(OFF[i]) for i, n in enumerate(NAMES)}


def kmajor(w):
    M = w.shape[1]
    return np.ascontiguousarray(w.reshape(16, 128, M).transpose(1, 0, 2).reshape(128, 16 * M))


def xblocks(xT):
    D, S = xT.shape
    NB = S // 256
    xp = np.zeros((D, S + 4), xT.dtype)
    xp[:, 2:S + 2] = xT
    out = np.empty((NB, 128, 16 * 260), xT.dtype)
    for b in range(NB):
        blk = xp[:, 256 * b:256 * b + 260]
        out[b] = blk.reshape(16, 128, 260).transpose(1, 0, 2).reshape(128, 16 * 260)
    return out


def unit_ids(p):
    return dict(hA=p, hB=4 + p // 2, half=p % 2, gh=p, kv=p // 2, qhalf=p % 2)


def mixer_weights(w_in, dn_conv, a_log, dt_bias, qn_g, kn_g, gla_up, gla_up_b, p):
    u = unit_ids(p)
    hA, hB, half, gh = u["hA"], u["hB"], u["half"], u["gh"]
    c = COL
    def cols(name, a, n):
        return w_in[:, c[name] + a:c[name] + a + n]
    fm = np.concatenate([cols("dq", hA * 128, 128), cols("dk", hA * 128, 128), cols("dv", hA * 128, 128),
                         cols("dq", hB * 128, 128), cols("dk", hB * 128, 128), cols("dv", hB * 128 + half * 64, 64),
                         cols("gq", gh * 64, 64), cols("gkk", gh * 64, 64), cols("lr_f", 0, 16), cols("lr_b", 0, 16)], axis=1)
    tm = np.concatenate([cols("gv", gh * 128, 128),
                         cols("a_f", hA, 1), cols("a_f", hB, 1), cols("b_f", hA, 1), cols("b_f", hB, 1),
                         cols("a_b", hA, 1), cols("a_b", hB, 1), cols("b_b", hA, 1), cols("b_b", hB, 1),
                         cols("dgate", hA * 128, 128), cols("dgate", hB * 128 + half * 64, 64), cols("ggate", gh * 128, 128)], axis=1)
    prm = np.zeros((128, 64), np.float32)
    chans = [(0 + hA * 128, 128), (768 + hA * 128, 128), (1536 + hA * 128, 128),
             (0 + hB * 128, 128), (768 + hB * 128, 128), (1536 + hB * 128 + half * 64, 64)]
    for ti, (c0, M) in enumerate(chans):
        prm[0:M, 5 * ti:5 * ti + 5] = dn_conv[:, c0:c0 + M].T
    prm[:, 30:34] = np.array([a_log[0, hA], a_log[0, hB], a_log[1, hA], a_log[1, hB]])[None, :]
    prm[:, 34:38] = np.array([dt_bias[0, hA], dt_bias[0, hB], dt_bias[1, hA], dt_bias[1, hB]])[None, :]
    prm[:, 38] = qn_g
    prm[:, 39] = kn_g
    prm[0:64, 40] = gla_up_b[0, gh * 64:(gh + 1) * 64]
    prm[0:64, 41] = gla_up_b[1, gh * 64:(gh + 1) * 64]
    up = np.concatenate([gla_up[0][:, gh * 64:(gh + 1) * 64], gla_up[1][:, gh * 64:(gh + 1) * 64]], axis=1)
    return dict(wfm=kmajor(np.ascontiguousarray(fm)), wtm=kmajor(np.ascontiguousarray(tm)), prm=prm, glaup=np.ascontiguousarray(up.astype(np.float32)))


def rope_tables(S):
    t = np.arange(S)
    row = (t // 64).astype(np.float32)
    col = (t % 64).astype(np.float32)
    inv = (10000.0 ** (-np.arange(0, 64, 2, dtype=np.float32) / 64)).astype(np.float32)
    cos = np.empty((128, S), np.float32); sin = np.empty((128, S), np.float32)
    for d in range(128):
        pos = row if d < 64 else col
        ang = (pos * inv[d % 32]).astype(np.float32)
        cos[d] = np.cos(ang); sin[d] = np.sin(ang)
    return cos, sin


def att_weights(w_in, p):
    u = unit_ids(p)
    kv = u["kv"]
    c = COL
    fa = np.concatenate([w_in[:, c["aq"] + (3 * kv + g) * 128:c["aq"] + (3 * kv + g + 1) * 128] for g in range(3)]
                        + [w_in[:, c["ak"] + kv * 128:c["ak"] + (kv + 1) * 128]], axis=1)
    ta = w_in[:, c["av"] + kv * 128:c["av"] + (kv + 1) * 128]
    return dict(wfa=kmajor(np.ascontiguousarray(fa)), wta=kmajor(np.ascontiguousarray(ta)))


import ml_dtypes


def build_dn(S, xdt):
    p = PB(); nc = p.nc
    NB = S // 256
    ext = lambda n, s, d: nc.dram_tensor(n, s, d, kind="ExternalInput").ap()
    out = lambda n, s: nc.dram_tensor(n, s, F32, kind="ExternalOutput").ap()
    xb = ext("xb", [NB, 128, 16 * 260], xdt)
    wfm = ext("wfm", [128, 16 * FM1], F32); wtm = ext("wtm", [128, 16 * TM1], F32)
    prm = ext("prm", [128, 64], F32); glaup = ext("glaup", [16, 128], F32); cst = ext("cst", [128, len(CN) * 128], F32)
    oA = out("oA", [S, 128]); oB = out("oB", [S, 64]); ogl = out("ogl", [S, 128]); gts = out("gts", [S, 320])
    ofw = nc.dram_tensor("ofw", [S, 320], F32).ap()
    phase_dngla(p, S, xdt, xb, wfm, wtm, prm, glaup, cst, oA, oB, ogl, gts, ofw, chdt=F32)
    p.finish()
    return p.emit()


def build_mix(S, xdt):
    p = PB(); nc = p.nc
    NB = S // 256; NQB = NB // 2; SQ = S // 2
    ext = lambda n, s, d: nc.dram_tensor(n, s, d, kind="ExternalInput").ap()
    out = lambda n, s: nc.dram_tensor(n, s, F32, kind="ExternalOutput").ap()
    xb = ext("xb", [NB, 128, 16 * 260], xdt); xq = ext("xq", [NQB, 128, 16 * 260], xdt)
    wfm = ext("wfm", [128, 16 * FM1], F32); wtm = ext("wtm", [128, 16 * TM1], F32)
    prm = ext("prm", [128, 64], F32); glaup = ext("glaup", [16, 128], F32); cst = ext("cst", [128, len(CN) * 128], F32)
    wfa = ext("wfa", [128, 16 * 512], F32); wta = ext("wta", [128, 16 * 128], F32)
    cosk = ext("cosk", [128, S], F32); sink = ext("sink", [128, S], F32)
    cosq = ext("cosq", [128, SQ], F32); sinq = ext("sinq", [128, SQ], F32)
    oA = out("oA", [S, 128]); oB = out("oB", [S, 64]); ogl = out("ogl", [S, 128]); gts = out("gts", [S, 320]); oat = out("oat", [SQ, 384])
    ofw = nc.dram_tensor("ofw", [S, 320], F32).ap()
    banks = [p.ps(f"bank{i}", [128, 512], F32) for i in range(8)]
    p.push()
    phase_dngla(p, S, xdt, xb, wfm, wtm, prm, glaup, cst, oA, oB, ogl, gts, ofw, chdt=F32, banks=banks)
    p.pop()
    p.push()
    phase_att(p, S, xdt, banks, xb, xq, wfa, wta, prm, cst, cosk, sink, cosq, sinq, oat)
    p.pop()
    p.finish()
    return p.emit()


_NC = {}


def _get(key, fn):
    if key not in _NC:
        _NC[key] = fn()
    return _NC[key]


import time as _time
from sys import stderr as _stderr
_T0 = [_time.time()]


def _log(msg):
    if os.environ.get("K_LOG"):
        print(f"[k {_time.time() - _T0[0]:7.1f}s] {msg}", file=_stderr, flush=True)


def _run(nc, maps):
    t = _time.time()
    r = run_bass_kernel_spmd(nc, maps, core_ids=list(range(len(maps)))).results
    _log(f"launch done in {_time.time() - t:.1f}s")
    return r


def kernel(x, w_in, dn_conv, dn_a_log, dn_dt_bias, dn_norm_g, att_qn_g, att_kn_g, gla_up, gla_up_b, gla_norm_g, w_out,
           ln1_g, ln1_b, ln2_g, ln2_b, ffn_w_gate, ffn_w_up, ffn_w_down, router_w, exp_w_gate, exp_w_up, exp_w_down):
    f32 = np.float32
    x = np.asarray(x, f32)
    B, S, D = x.shape
    L = w_in.shape[0]
    T = B * S; NCORE = 8; TC = T // NCORE; TT = TC // 128
    NB = S // 256; SQ = S // 2
    DFF = ffn_w_gate.shape[2]; FD = DFF // NCORE
    TP = 1024 if T % 1024 == 0 and TC >= 1024 else 512
    NP = T // TP
    cst = make_consts(); cos, sin = rope_tables(S); ident = np.eye(128, dtype=f32)
    xcur = np.ascontiguousarray(x.reshape(T, D))
    xT_b = [np.ascontiguousarray(x[b].T) for b in range(B)]
    bdt = {True: F32, False: BF16}
    _T0[0] = _time.time()
    for l in range(L):
        _log(f'layer {l}')
        xdt = F32 if l == 0 else BF16
        xbs = [xblocks(xT_b[b]) for b in range(B)]
        mws = [mixer_weights(np.asarray(w_in[l]), np.asarray(dn_conv[l]), np.asarray(dn_a_log[l]), np.asarray(dn_dt_bias[l]),
                             np.asarray(att_qn_g[l]), np.asarray(att_kn_g[l]), np.asarray(gla_up[l]), np.asarray(gla_up_b[l]), p_) for p_ in range(4)]
        _log('DeltaNet + GLA launch')
        _log('attention launch')
        nc_mx = _get(("mix", S, l == 0), lambda: build_mix(S, xdt))
        maps = []
        for c in range(NCORE):
            b, p_ = divmod(c, 4); qh = p_ % 2
            maps.append(dict(xb=xbs[b], xq=np.ascontiguousarray(xbs[b][qh * NB // 2:(qh + 1) * NB // 2]), cst=cst, **mws[p_],
                             cosk=cos, sink=sin, cosq=np.ascontiguousarray(cos[:, qh * SQ:(qh + 1) * SQ]),
                             sinq=np.ascontiguousarray(sin[:, qh * SQ:(qh + 1) * SQ]), **att_weights(np.asarray(w_in[l]), p_)))
        r_at = _run(nc_mx, maps)
        r_dn = r_at
        del xbs, maps
        o = np.empty((T, 2048), f32); gt = np.empty((T, 1280), f32)
        for c in range(NCORE):
            b, p_ = divmod(c, 4); hB = 4 + p_ // 2; half = p_ % 2; kv = p_ // 2; qh = p_ % 2
            rows = slice(b * S, (b + 1) * S)
            rd = r_dn[c]
            o[rows, p_ * 128:(p_ + 1) * 128] = rd["oA"]
            o[rows, hB * 128 + half * 64:hB * 128 + half * 64 + 64] = rd["oB"]
            o[rows, 1536 + p_ * 128:1536 + (p_ + 1) * 128] = rd["ogl"]
            o[b * S + qh * SQ:b * S + (qh + 1) * SQ, 768 + 3 * kv * 128:768 + 3 * kv * 128 + 384] = r_at[c]["oat"]
            g_ = rd["gts"]
            gt[rows, p_ * 128:(p_ + 1) * 128] = g_[:, 0:128]
            gt[rows, hB * 128 + half * 64:hB * 128 + half * 64 + 64] = g_[:, 128:192]
            gt[rows, 768 + p_ * 128:768 + (p_ + 1) * 128] = g_[:, 192:320]
        del r_dn, r_at
        _log('post launch (token para')
        moe = (l % 2 == 1); j = l // 2
        nc_po = _get(("post", TT, moe), lambda: build_post(TT, moe))
        wout_k = kmajor(np.asarray(w_out[l], f32))
        nrm = np.tile(np.concatenate([np.asarray(dn_norm_g[l], f32), np.asarray(gla_norm_g[l], f32)])[None], (128, 1))
        ln1 = np.tile(np.concatenate([np.asarray(ln1_g[l], f32), np.asarray(ln1_b[l], f32)])[None], (128, 1))
        maps = []
        for c in range(NCORE):
            sl = slice(c * TC, (c + 1) * TC)
            m = dict(o=o[sl], gt=gt[sl], x=xcur[sl], wout=wout_k, nrm=nrm, ln=ln1, ident=ident)
            if moe:
                m["rw"] = kmajor(np.asarray(router_w[j], f32))
            maps.append(m)
        r_po = _run(nc_po, maps)
        del o, gt, maps
        x1 = [r_po[c]["x1"] for c in range(NCORE)]
        gates = [r_po[c]["gates"] for c in range(NCORE)] if moe else None
        x1T = np.stack([np.asarray(r_po[c]["x1T"]).reshape(TT, 128, 16, 128) for c in range(NCORE)]).reshape(NP, TP // 128, 128, 16, 128)
        xT_all = np.ascontiguousarray(x1T.transpose(0, 2, 3, 1, 4).reshape(NP, 128, 16 * TP))
        del r_po, x1T
        _log('FFN launch (expert / ff')
        F_ = DFF if moe else FD
        gc = 2 if moe else 1
        nc_ff = _get(("ffn", NP, F_, gc, TP), lambda: build_ffn(NP, F_, gc, TP))
        maps = []
        for e in range(NCORE):
            if moe:
                wg_ = np.asarray(exp_w_gate[j][e], f32); wu_ = np.asarray(exp_w_up[j][e], f32); wd_ = np.asarray(exp_w_down[j][e], f32)
            else:
                wg_ = np.asarray(ffn_w_gate[j][:, e * FD:(e + 1) * FD], f32); wu_ = np.asarray(ffn_w_up[j][:, e * FD:(e + 1) * FD], f32)
                wd_ = np.asarray(ffn_w_down[j][e * FD:(e + 1) * FD, :], f32)
            wdl = np.ascontiguousarray(wd_.reshape(F_ // 128, 128, 2048).transpose(1, 0, 2).reshape(128, (F_ // 128) * 2048))
            maps.append(dict(xT=xT_all, wg=kmajor(np.ascontiguousarray(wg_)), wu=kmajor(np.ascontiguousarray(wu_)), wd=wdl))
        r_ff = _run(nc_ff, maps)
        del maps, xT_all
        _log('combine launch (token p')
        nc_cb = _get(("comb", TT, moe), lambda: build_combine(TT, moe))
        ln2 = np.tile(np.concatenate([np.asarray(ln2_g[l], f32), np.asarray(ln2_b[l], f32)])[None], (128, 1))
        maps = []
        for c in range(NCORE):
            sl = slice(c * TC, (c + 1) * TC)
            m = dict(x1=x1[c], y=np.stack([r_ff[e]["y"][sl] for e in range(NCORE)]), ln=ln2, ident=ident)
            if moe:
                m["gates"] = gates[c]
            maps.append(m)
        del r_ff
        r_cb = _run(nc_cb, maps)
        del maps
        xcur = np.concatenate([r_cb[c]["x2"] for c in range(NCORE)], axis=0)
        xT_b = []
        for b in range(B):
            cols = []
            for c in range(4 * b, 4 * b + 4):
                a = np.asarray(r_cb[c]["x2T"]).reshape(TT, 128, 16, 128).transpose(2, 1, 0, 3).reshape(2048, TC)
                cols.append(a)
            xT_b.append(np.ascontiguousarray(np.concatenate(cols, axis=1)))
        del r_cb
    return np.ascontiguousarray(xcur.reshape(B, S, D).astype(f32))
```

```python
import os


import numpy as np
import concourse.bass as bass
import concourse.mybir as mybir
from concourse.bass_utils import run_bass_kernel_spmd
from contextlib import ExitStack

F32 = mybir.dt.float32
BF16 = mybir.dt.bfloat16
I32 = mybir.dt.int32
AF = mybir.ActivationFunctionType
ALU = mybir.AluOpType
AX = mybir.AxisListType


class Buf:
    __slots__ = ("t", "w", "r", "name", "dsem")

    def __init__(self, t, name):
        self.t = t
        self.name = name
        self.w = None
        self.r = {}
        self.dsem = None

    def __getitem__(self, idx):
        return self.t[idx]


class Reg:
    __slots__ = ("b", "c0", "name")

    def __init__(self, b, c0, name):
        self.b = b
        self.c0 = c0
        self.name = name

    def __getitem__(self, idx):
        rs, cs = idx
        a = self.c0 + (cs.start or 0)
        z = self.c0 + cs.stop
        return self.b.t[rs, a:z]

    w = property(lambda self: self.b.w, lambda self, v: setattr(self.b, "w", v))
    r = property(lambda self: self.b.r, lambda self, v: setattr(self.b, "r", v))
    dsem = property(lambda self: self.b.dsem, lambda self, v: setattr(self.b, "dsem", v))


class PB:
    ENGS = ("tensor", "vector", "scalar", "gpsimd", "sync")

    def __init__(self, same_engine_sync=True):
        self.nc = bass.Bass("TRN2", target_bir_lowering=False)
        self.es = ExitStack()
        self.base_es = self.es
        self.scopes = []
        self.streams = {e: [] for e in self.ENGS}
        self.sems = {}
        self.semcount = {}
        self.known = {e: {} for e in self.ENGS}
        self.same_engine_sync = same_engine_sync
        for e in ("tensor", "vector", "scalar", "gpsimd"):
            self._mksem("E_" + e)
        self.nbuf = 0
        self.nops = 0
        import os
        self.limit = int(os.environ.get('PB_LIMIT', '1000000000'))
        self.trace_op = int(os.environ.get('PB_TRACE', '-1'))
        self.nopool = os.environ.get('PB_NOPOOL') == '1'
        self.ninstr = {}

    def _mksem(self, key):
        self.sems[key] = self.base_es.enter_context(self.nc.semaphore(key))
        self.semcount[key] = 0

    def sb(self, name, shape, dtype):
        t = self.es.enter_context(self.nc.sbuf_tensor(name, list(shape), dtype))
        return Buf(t, name)

    def ps(self, name, shape, dtype=F32):
        t = self.es.enter_context(self.nc.psum_tensor(name, list(shape), dtype))
        return Buf(t, name)

    def dram(self, name, shape, dtype, kind="Internal"):
        t = self.nc.dram_tensor(name, list(shape), dtype, kind=kind)
        return Buf(t.ap(), name)

    def view(self, buf_or_ap, name=None):
        self.nbuf += 1
        return Buf(buf_or_ap, name or f"v{self.nbuf}")

    def push(self):
        self.scopes.append(self.es)
        self.es = ExitStack()

    def pop(self):
        for eng in self.ENGS:
            e = getattr(self.nc, eng)
            kn = self.known[eng]
            for k, v in self.semcount.items():
                if v > 0 and kn.get(k, 0) < v:
                    e.wait_ge(self.sems[k], v)
                    kn[k] = v
        self.es.close()
        self.es = self.scopes.pop()

    def _deps(self, stream, reads, writes, skipkey=None):
        need = {}
        def add(k, v):
            if v > need.get(k, 0):
                need[k] = v
        for b in reads:
            if b.w is not None:
                add(*b.w)
        for b in writes:
            if b.w is not None and b.w[0] != skipkey:
                add(*b.w)
            for k, v in b.r.items():
                add(k, v)
        own = "E_" + stream
        kn = self.known[stream]
        out = []
        for k, v in need.items():
            if k == own and (not self.same_engine_sync or stream == "tensor"):
                continue
            if kn.get(k, 0) >= v:
                continue
            kn[k] = v
            out.append((k, v))
        return out

    def op(self, eng, fn, reads=(), writes=(), inc=True):
        self.nops += 1
        if self.nops > self.limit:
            return 0
        if self.nops == self.trace_op:
            import traceback; traceback.print_stack(limit=4)
        if eng == 'gpsimd' and self.nopool:
            eng = 'vector'
        waits = self._deps(eng, reads, writes)
        key = "E_" + eng
        if inc:
            self.semcount[key] += 1
            val = self.semcount[key]
        else:
            val = self.semcount[key] + 1
        self._emit(eng, waits, fn, key, 1 if inc else 0)
        for b in reads:
            if b.r.get(key, 0) < val:
                b.r[key] = val
        for b in writes:
            b.w = (key, val)
            b.r = {}
        return val

    def _emit(self, eng, waits, fn, key, inc):
        e = getattr(self.nc, eng)
        for k, v in waits:
            e.wait_ge(self.sems[k], v)
        self.ninstr[eng] = self.ninstr.get(eng, 0) + 1
        if fn is not None:
            ins = fn(e)
            if inc:
                ins.then_inc(self.sems[key], inc)

    def dma(self, queue, out, in_, reads=(), writes=(), sembuf=None, **kw):
        self.nops += 1
        if self.nops > self.limit:
            return 0
        if self.nops == self.trace_op:
            import traceback; traceback.print_stack(limit=4)
        sb_ = sembuf or (writes[0] if writes else reads[0])
        if sb_.dsem is None:
            sb_.dsem = "D_%d_%s" % (len(self.sems), sb_.name)
            self._mksem(sb_.dsem)
        key = sb_.dsem
        waits = self._deps(queue, reads, writes, skipkey=key)
        self.semcount[key] += 16
        val = self.semcount[key]
        fn = lambda e, out=out, in_=in_, kw=kw: e.dma_start(out=out, in_=in_, **kw)
        self._emit(queue, waits, fn, key, 16)
        for b in reads:
            if b.r.get(key, 0) < val:
                b.r[key] = val
        for b in writes:
            b.w = (key, val)
            b.r = {}
        return val

    def finish(self):
        self.final_waits = [(k, v) for k, v in self.semcount.items() if v > 0]

    def emit(self):
        nc = self.nc
        sems = self.sems
        fw_ = self.final_waits
        with nc.Block() as block:
            def run(e, name):
                for k, v in fw_:
                    e.wait_ge(sems[k], v)

            @block.tensor
            def _(e):
                run(e, "tensor")

            @block.vector
            def _(e):
                run(e, "vector")

            @block.scalar
            def _(e):
                run(e, "scalar")

            @block.gpsimd
            def _(e):
                run(e, "gpsimd")

            @block.sync
            def _(e):
                run(e, "sync")
        self.base_es.close()
        return nc


FM1 = 864
TM1 = 456
FMO = dict(qA=(0,128), kA=(128,128), vA=(256,128), qB=(384,128), kB=(512,128), vB=(640,64),
           gq=(704,64), gk=(768,64), lrf=(832,16), lrb=(848,16))
CN = ["IDENT", "ONES", "NEGONES", "TRI0", "TRI1", "NEGM0", "NEGM1", "STR0", "STR1", "GM0", "GM1", "ROT"]
EPS = 1e-6
import os
EVAC_ACT = True


def make_consts():
    i = np.arange(128)
    c = {}
    c["IDENT"] = np.eye(128)
    c["ONES"] = np.ones((128, 128))
    c["NEGONES"] = -np.ones((128, 128))
    c["TRI0"] = (i[:, None] <= i[None, :]).astype(np.float64)
    c["TRI1"] = (i[:, None] >= i[None, :]).astype(np.float64)
    c["NEGM0"] = np.where(i[:, None] >= i[None, :], 0.0, -30000.0)
    c["NEGM1"] = np.where(i[:, None] <= i[None, :], 0.0, -30000.0)
    c["STR0"] = (i[:, None] > i[None, :]).astype(np.float64)
    c["STR1"] = (i[:, None] < i[None, :]).astype(np.float64)
    c["GM0"] = (i[None, :] >= i[:, None]).astype(np.float64)
    c["GM1"] = (i[None, :] <= i[:, None]).astype(np.float64)
    rot = np.zeros((128, 128))
    for m in range(128):
        if (m % 64) < 32:
            rot[m + 32, m] = -1.0
        else:
            rot[m - 32, m] = 1.0
    c["ROT"] = rot
    return np.concatenate([c[n] for n in CN], axis=1).astype(np.float32)


class Ring:
    def __init__(self, bufs):
        self.bufs = bufs
        self.i = 0

    def next(self):
        b = self.bufs[self.i % len(self.bufs)]
        self.i += 1
        return b


def phase_dngla(p, S, xdt, xb, wfm, wtm, prm, glaup, cst, oA, oB, ogl, gts, ofw, chdt=BF16, scdt=BF16, predt=BF16, banks=None):
    nc = p.nc
    NT = S // 128
    NB = S // 256
    wfm_sb = p.sb("wfm_sb", [128, 16, FM1], BF16)
    wtm_sb = p.sb("wtm_sb", [128, 16, TM1], BF16)
    prm_sb = p.sb("prm_sb", [128, 64], F32)
    up_sb = p.sb("up_sb", [16, 128], BF16)
    cst_sb = p.sb("cst_sb", [128, len(CN) * 128], F32)
    idb = p.sb("idb", [128, 128], BF16)
    idf = p.sb("idf", [128, 128], F32)
    IDN = {BF16: idb, F32: idf}
    C = {n: cst_sb[:, i * 128:(i + 1) * 128] for i, n in enumerate(CN)}
    for k in range(16):
        p.dma("gpsimd", wfm_sb[:, k, :], wfm[:, k * FM1:(k + 1) * FM1], writes=[wfm_sb])
        p.dma("gpsimd", wtm_sb[:, k, :], wtm[:, k * TM1:(k + 1) * TM1], writes=[wtm_sb])
    p.dma("sync", prm_sb[:], prm[:, :], writes=[prm_sb])
    p.dma("gpsimd", up_sb[:], glaup[:, :], writes=[up_sb])
    p.dma("sync", cst_sb[:], cst[:, :], writes=[cst_sb])
    p.op("vector", lambda e: e.tensor_copy(out=idb[:], in_=C["IDENT"]), reads=[cst_sb], writes=[idb])
    p.op("vector", lambda e: e.tensor_copy(out=idf[:], in_=C["IDENT"]), reads=[cst_sb], writes=[idf])
    expA = p.sb("expA", [128, 4], F32)
    p.op("scalar", lambda e: e.activation(out=expA[:], in_=prm_sb[:, 30:34], func=AF.Exp), reads=[prm_sb], writes=[expA])

    if banks is None:
        banks = [p.ps(f"bank{i}", [128, 512], F32) for i in range(8)]
    PR = Ring([banks[0], banks[1], banks[2]])
    qregs = []
    for b in (3, 4, 5):
        for q in range(4):
            qregs.append(Reg(banks[b], q * 128, f"q{b}_{q}"))
    QR = Ring(qregs)
    hregs = []
    for b in (6, 7):
        for h in range(2):
            hregs.append(Reg(banks[b], h * 256, f"h{b}_{h}"))
    HR = Ring(hregs)

    ofw_t = [p.view(ofw[t * 128:(t + 1) * 128, :], f'ofw{t}') for t in range(NT)]
    evac_i = [0]

    def evac(out_buf, out_ap, in_buf, in_ap, extra_reads=()):
        evac_i[0] += 1
        if evac_i[0] % 2 or EVAC_ACT:
            p.op("scalar", lambda e: e.copy(out=out_ap, in_=in_ap), reads=[in_buf, *extra_reads], writes=[out_buf])
        else:
            p.op("vector", lambda e: e.tensor_scalar(out=out_ap, in0=in_ap, scalar1=1.0, scalar2=None, op0=ALU.mult), reads=[in_buf, *extra_reads], writes=[out_buf])

    def transp(src_buf, src_ap, M, dst_buf, dst_ap):
        q = QR.next()
        idc = IDN[src_buf.t.dtype]
        p.op("tensor", lambda e: e.matmul(q[0:M, 0:128], lhsT=src_ap, rhs=idc[:], start=True, stop=True),
             reads=[src_buf, idc], writes=[q])
        evac(dst_buf, dst_ap, q, q[0:M, 0:128])

    slots = [dict(name="A", q="qA", k="kA", v="vA", dvw=128, ci=0, oc=0),
             dict(name="B", q="qB", k="kB", v="vB", dvw=64, ci=3, oc=128)]
    CH = []
    for tt in range(2):
        for s in slots:
            n = f"{tt}{s['name']}"
            dvw = s["dvw"]
            ch = dict(tt=tt, s=s, dvw=dvw)
            for nm, shp, dt in [("qn", [128, 128], predt), ("kn", [128, 128], predt), ("vt", [128, dvw], predt),
                                ("kT", [128, 128], predt), ("qT", [128, 128], predt), ("G1", [128, 128], F32),
                                ("Dm", [128, 128], F32), ("D", [128, 128], F32), ("t1", [128, 128], F32),
                                ("N0", [128, 128], chdt), ("N1", [128, 128], chdt), ("NT0", [128, 128], chdt),
                                ("NT1", [128, 128], chdt), ("R0", [128, dvw + 128], chdt), ("R1", [128, dvw + 128], chdt),
                                ("u", [128, dvw], F32), ("w", [128, 128], scdt), ("wT", [128, 128], scdt),
                                ("qkD", [128, 128], scdt), ("qkDT", [128, 128], scdt), ("qd", [128, 128], scdt),
                                ("qdT", [128, 128], scdt), ("kt", [128, 128], scdt), ("vn", [128, dvw], scdt),
                                ("ss", [128, 4], F32), ("osb", [128, dvw], F32)]:
                ch[nm] = p.sb(f"{nm}_{n}", shp, dt)
            CH.append(ch)
    for s in slots:
        s["S"] = p.sb(f"S_{s['name']}", [128, s["dvw"]], F32)
        s["Sb"] = p.sb(f"Sb_{s['name']}", [128, s["dvw"]], scdt)
        s["cq"] = p.sb(f"cq_{s['name']}", [128, 256], predt)
        s["ck"] = p.sb(f"ck_{s['name']}", [128, 256], predt)
        s["cv"] = p.sb(f"cv_{s['name']}", [128, 256], predt)
        s["acc"] = p.sb(f"acc_{s['name']}", [128, 256], F32)
    SC = []
    for tt in range(2):
        d = {}
        for nm in ["z", "t", "e", "l", "g", "beta", "gc", "gl", "egc", "etail", "egl", "nbeta", "bege", "tmp"]:
            d[nm] = p.sb(f"sc_{nm}{tt}", [128, 2], F32)
        d["tm"] = p.sb(f"tm{tt}", [128, TM1], F32)
        d["gvb"] = p.sb(f"gvb{tt}", [128, 128], BF16)
        d["ofl"] = p.sb(f"ofl{tt}", [128, 320], F32)
        SC.append(d)
    xblk = [p.sb(f"xblk{i}", [128, 16, 260], BF16) for i in range(2)]
    G = {}
    for nm, shp, dt in [("q", [64, 256], F32), ("k", [64, 256], F32), ("lr0", [16, 256], BF16), ("lr1", [16, 256], BF16),
                        ("zb", [64, 256], F32), ("ta", [64, 256], F32), ("gkk", [64, 256], F32), ("gc", [64, 256], F32),
                        ("ex", [64, 256], F32), ("qpos", [64, 256], BF16), ("kneg", [64, 256], BF16),
                        ("ktl", [64, 256], BF16), ("ones", [64, 256], F32), ("glc", [64, 2], F32), ("egl", [64, 2], F32),
                        ("S", [64, 128], F32), ("Sb", [64, 128], BF16)]:
        G[nm] = p.sb(f"gla_{nm}", shp, dt)
    GT = []
    for tt in range(2):
        d = {}
        for nm, shp, dt in [("AT", [128, 128], BF16), ("kt", [128, 64], BF16), ("osb", [128, 128], F32)]:
            d[nm] = p.sb(f"glat_{nm}{tt}", shp, dt)
        GT.append(d)
    p.op("gpsimd", lambda e: e.memset(G["ones"][:], 1.0), writes=[G["ones"]])

    def fm_proj(blk, name):
        m0, M = FMO[name]
        bank = PR.next()
        for k in range(16):
            p.op("tensor", lambda e, k=k: e.matmul(bank[0:M, 0:260], lhsT=wfm_sb[:, k, m0:m0 + M], rhs=blk[:, k, :],
                                                   start=(k == 0), stop=(k == 15)),
                 reads=[wfm_sb, blk], writes=[bank], inc=(k == 15))
        return bank, M

    for dirn in (0, 1):
        TRI = C[f"TRI{dirn}"]; NEGM = C[f"NEGM{dirn}"]; STR = C[f"STR{dirn}"]; GM = C[f"GM{dirn}"]
        for s in slots:
            p.op("gpsimd", lambda e, s=s: e.memset(s["S"][:], 0.0), writes=[s["S"]])
            p.op("gpsimd", lambda e, s=s: e.memset(s["Sb"][:], 0.0), writes=[s["Sb"]])
        p.op("gpsimd", lambda e: e.memset(G["S"][:], 0.0), writes=[G["S"]])
        p.op("gpsimd", lambda e: e.memset(G["Sb"][:], 0.0), writes=[G["Sb"]])
        blocks = range(NB) if dirn == 0 else range(NB - 1, -1, -1)
        for bi, b in enumerate(blocks):
            blk = xblk[bi % 2]
            if xdt == BF16:
                p.dma("sync", blk[:].rearrange("p k t -> p (k t)"), xb[b, :, :], writes=[blk])
            else:
                for k in range(16):
                    p.dma("gpsimd", blk[:, k, :], xb[b, :, k * 260:(k + 1) * 260], writes=[blk])
            tiles = (0, 1) if dirn == 0 else (1, 0)
            ncol = TM1 if dirn == 0 else 136
            for tt in (0, 1):
                bank = PR.next()
                for k in range(16):
                    p.op("tensor", lambda e, k=k, tt=tt, bank=bank: e.matmul(
                        bank[:, 0:ncol], lhsT=blk[:, k, 2 + 128 * tt:2 + 128 * (tt + 1)], rhs=wtm_sb[:, k, 0:ncol],
                        start=(k == 0), stop=(k == 15)), reads=[blk, wtm_sb], writes=[bank], inc=(k == 15))
                tm = SC[tt]["tm"]
                evac(tm, tm[:, 0:ncol], bank, bank[:, 0:ncol])
                p.op("gpsimd", lambda e, tt=tt, tm=tm: e.tensor_copy(out=SC[tt]["gvb"][:], in_=tm[:, 0:128]),
                     reads=[tm], writes=[SC[tt]["gvb"]])
                if dirn == 0:
                    t0 = b * 256 + tt * 128
                    p.dma("sync", gts[t0:t0 + 128, :], tm[:, 136:456], reads=[tm], sembuf=tm)
            for s in slots:
                for comp, dst in (("q", "cq"), ("k", "ck"), ("v", "cv")):
                    bank, M = fm_proj(blk, s[comp])
                    ti = s["ci"] + ("q", "k", "v").index(comp)
                    acc = s["acc"]
                    p.op("vector", lambda e, bank=bank, M=M, ti=ti, acc=acc: e.tensor_scalar(
                        out=acc[0:M, :], in0=bank[0:M, 0:256], scalar1=prm_sb[0:M, 5 * ti:5 * ti + 1], scalar2=None,
                        op0=ALU.mult), reads=[bank, prm_sb], writes=[acc])
                    for j in range(1, 5):
                        p.op("vector", lambda e, bank=bank, M=M, ti=ti, acc=acc, j=j: e.scalar_tensor_tensor(
                            out=acc[0:M, :], in0=bank[0:M, j:j + 256], scalar=prm_sb[0:M, 5 * ti + j:5 * ti + j + 1],
                            in1=acc[0:M, :], op0=ALU.mult, op1=ALU.add), reads=[bank, prm_sb, acc], writes=[acc])
                    d_ = s[dst]
                    p.op("scalar", lambda e, d_=d_, M=M, acc=acc: e.activation(out=d_[0:M, :], in_=acc[0:M, :], func=AF.Silu),
                         reads=[acc], writes=[d_])
            for tt in (0, 1):
                sc = SC[tt]; tm = sc["tm"]
                a_ap = tm[:, 128 + 4 * dirn:128 + 4 * dirn + 2]
                b_ap = tm[:, 128 + 4 * dirn + 2:128 + 4 * dirn + 4]
                dtb = prm_sb[:, 34 + 2 * dirn:36 + 2 * dirn]
                Aex = expA[:, 2 * dirn:2 * dirn + 2]
                p.op("vector", lambda e, sc=sc, a_ap=a_ap, dtb=dtb: e.tensor_tensor(out=sc["z"][:], in0=a_ap, in1=dtb, op=ALU.add),
                     reads=[tm, prm_sb], writes=[sc["z"]])
                p.op("scalar", lambda e, sc=sc: e.activation(out=sc["t"][:], in_=sc["z"][:], func=AF.Abs), reads=[sc["z"]], writes=[sc["t"]])
                p.op("scalar", lambda e, sc=sc: e.activation(out=sc["e"][:], in_=sc["t"][:], func=AF.Exp, scale=-1.0), reads=[sc["t"]], writes=[sc["e"]])
                p.op("scalar", lambda e, sc=sc: e.activation(out=sc["l"][:], in_=sc["e"][:], func=AF.Ln, bias=1.0), reads=[sc["e"]], writes=[sc["l"]])
                p.op("vector", lambda e, sc=sc: e.scalar_tensor_tensor(out=sc["tmp"][:], in0=sc["z"][:], scalar=0.0, in1=sc["l"][:], op0=ALU.max, op1=ALU.add),
                     reads=[sc["z"], sc["l"]], writes=[sc["tmp"]])
                p.op("vector", lambda e, sc=sc, Aex=Aex: e.scalar_tensor_tensor(out=sc["g"][:], in0=sc["tmp"][:], scalar=-1.0, in1=Aex, op0=ALU.mult, op1=ALU.mult),
                     reads=[sc["tmp"], expA], writes=[sc["g"]])
                p.op("scalar", lambda e, sc=sc, b_ap=b_ap: e.activation(out=sc["beta"][:], in_=b_ap, func=AF.Sigmoid), reads=[tm], writes=[sc["beta"]])
                q1 = QR.next()
                p.op("tensor", lambda e, q1=q1, sc=sc: e.matmul(q1[:, 0:2], lhsT=TRI, rhs=sc["g"][:], start=True, stop=True), reads=[cst_sb, sc["g"]], writes=[q1])
                q2 = QR.next()
                p.op("tensor", lambda e, q2=q2, sc=sc: e.matmul(q2[:, 0:2], lhsT=C["ONES"], rhs=sc["g"][:], start=True, stop=True), reads=[cst_sb, sc["g"]], writes=[q2])
                p.op("vector", lambda e, q1=q1, sc=sc: e.tensor_copy(out=sc["gc"][:], in_=q1[:, 0:2]), reads=[q1], writes=[sc["gc"]])
                p.op("vector", lambda e, q2=q2, sc=sc: e.tensor_copy(out=sc["gl"][:], in_=q2[:, 0:2]), reads=[q2], writes=[sc["gl"]])
                p.op("scalar", lambda e, sc=sc: e.activation(out=sc["egc"][:], in_=sc["gc"][:], func=AF.Exp), reads=[sc["gc"]], writes=[sc["egc"]])
                p.op("scalar", lambda e, sc=sc: e.activation(out=sc["egl"][:], in_=sc["gl"][:], func=AF.Exp), reads=[sc["gl"]], writes=[sc["egl"]])
                p.op("vector", lambda e, sc=sc: e.tensor_tensor(out=sc["tmp"][:], in0=sc["gl"][:], in1=sc["gc"][:], op=ALU.subtract), reads=[sc["gl"], sc["gc"]], writes=[sc["tmp"]])
                p.op("scalar", lambda e, sc=sc: e.activation(out=sc["etail"][:], in_=sc["tmp"][:], func=AF.Exp), reads=[sc["tmp"]], writes=[sc["etail"]])
                p.op("vector", lambda e, sc=sc: e.tensor_scalar(out=sc["nbeta"][:], in0=sc["beta"][:], scalar1=-1.0, scalar2=None, op0=ALU.mult), reads=[sc["beta"]], writes=[sc["nbeta"]])
                p.op("vector", lambda e, sc=sc: e.tensor_tensor(out=sc["bege"][:], in0=sc["beta"][:], in1=sc["egc"][:], op=ALU.mult), reads=[sc["beta"], sc["egc"]], writes=[sc["bege"]])
            chs = CH
            def col(sc, nm, si):
                return sc[nm][:, si:si + 1]
            for ch in chs:
                s = ch["s"]; tt = ch["tt"]; dvw = ch["dvw"]
                for src, dstn, M in ((s["cq"], "qn", 128), (s["ck"], "kn", 128), (s["cv"], "vt", dvw)):
                    q = QR.next()
                    Mi = 128 if dstn != "vt" else dvw
                    p.op("tensor", lambda e, q=q, src=src, tt=tt, Mi=Mi: e.matmul(q[:, 0:Mi], lhsT=src[0:Mi, 128 * tt:128 * tt + 128], rhs=IDN[predt][0:Mi, 0:Mi], start=True, stop=True),
                         reads=[src, IDN[predt]], writes=[q])
                    if dstn == "vt":
                        evac(ch["vt"], ch["vt"][:], q, q[:, 0:dvw])
                    else:
                        ci = 0 if dstn == "qn" else 1
                        p.op("scalar", lambda e, ch=ch, q=q, ci=ci: e.activation(out=ch["t1"][:], in_=q[:, 0:128], func=AF.Square, accum_out=ch["ss"][:, ci:ci + 1]),
                             reads=[q], writes=[ch["t1"], ch["ss"]])
                        p.op("scalar", lambda e, ch=ch, ci=ci: e.activation(out=ch["ss"][:, ci + 2:ci + 3], in_=ch["ss"][:, ci:ci + 1], func=AF.Sqrt, bias=EPS, scale=1.0),
                             reads=[ch["ss"]], writes=[ch["ss"]])
                        p.op("vector", lambda e, ch=ch, ci=ci: e.reciprocal(out=ch["ss"][:, ci:ci + 1], in_=ch["ss"][:, ci + 2:ci + 3]),
                             reads=[ch["ss"]], writes=[ch["ss"]])
                        sc2 = (128.0 ** -0.5) if dstn == "qn" else 1.0
                        p.op("vector", lambda e, ch=ch, q=q, ci=ci, dstn=dstn, sc2=sc2: e.tensor_scalar(
                            out=ch[dstn][:], in0=q[:, 0:128], scalar1=ch["ss"][:, ci:ci + 1], scalar2=sc2, op0=ALU.mult, op1=ALU.mult),
                            reads=[q, ch["ss"]], writes=[ch[dstn]])
            for ch in chs:
                transp(ch["kn"], ch["kn"][:], 128, ch["kT"], ch["kT"][:])
                transp(ch["qn"], ch["qn"][:], 128, ch["qT"], ch["qT"][:])
            for ch in chs:
                sc = SC[ch["tt"]]; si = 0 if ch["s"]["name"] == "A" else 1
                p.op("gpsimd", lambda e, ch=ch, sc=sc, si=si: e.tensor_scalar(out=ch["G1"][:], in0=TRI, scalar1=sc["g"][:, si:si + 1], scalar2=None, op0=ALU.mult),
                     reads=[cst_sb, sc["g"]], writes=[ch["G1"]])
            for ch in chs:
                q = QR.next(); ch["qE"] = q
                p.op("tensor", lambda e, ch=ch, q=q: e.matmul(q[:, 0:128], lhsT=ch["G1"][:], rhs=C["ONES"], start=True, stop=False), reads=[ch["G1"], cst_sb], writes=[q])
                p.op("tensor", lambda e, ch=ch, q=q: e.matmul(q[:, 0:128], lhsT=C["NEGONES"], rhs=ch["G1"][:], start=False, stop=True), reads=[ch["G1"], cst_sb], writes=[q])
            for ch in chs:
                q = ch["qE"]
                p.op("vector", lambda e, ch=ch, q=q: e.scalar_tensor_tensor(out=ch["Dm"][:], in0=q[:, 0:128], scalar=0.0, in1=NEGM, op0=ALU.min, op1=ALU.add),
                     reads=[q, cst_sb], writes=[ch["Dm"]])
                p.op("scalar", lambda e, ch=ch: e.activation(out=ch["D"][:], in_=ch["Dm"][:], func=AF.Exp), reads=[ch["Dm"]], writes=[ch["D"]])
            for ch in chs:
                sc = SC[ch["tt"]]; si = 0 if ch["s"]["name"] == "A" else 1
                q = QR.next()
                p.op("tensor", lambda e, ch=ch, q=q: e.matmul(q[:, 0:128], lhsT=ch["kT"][:], rhs=ch["kT"][:], start=True, stop=True), reads=[ch["kT"]], writes=[q])
                p.op("vector", lambda e, ch=ch, q=q: e.tensor_tensor(out=ch["t1"][:], in0=q[:, 0:128], in1=ch["D"][:], op=ALU.mult), reads=[q, ch["D"]], writes=[ch["t1"]])
                p.op("vector", lambda e, ch=ch, sc=sc, si=si: e.scalar_tensor_tensor(out=ch["N0"][:], in0=ch["t1"][:], scalar=sc["nbeta"][:, si:si + 1], in1=STR, op0=ALU.mult, op1=ALU.mult),
                     reads=[ch["t1"], sc["nbeta"], cst_sb], writes=[ch["N0"]])
                q2 = QR.next()
                p.op("tensor", lambda e, ch=ch, q2=q2: e.matmul(q2[:, 0:128], lhsT=ch["qT"][:], rhs=ch["kT"][:], start=True, stop=True), reads=[ch["qT"], ch["kT"]], writes=[q2])
                p.op("vector", lambda e, ch=ch, q2=q2: e.tensor_tensor(out=ch["qkD"][:], in0=q2[:, 0:128], in1=ch["D"][:], op=ALU.mult), reads=[q2, ch["D"]], writes=[ch["qkD"]])
            for ch in chs:
                transp(ch["N0"], ch["N0"][:], 128, ch["NT0"], ch["NT0"][:])
                transp(ch["qkD"], ch["qkD"][:], 128, ch["qkDT"], ch["qkDT"][:])
            for ch in chs:
                sc = SC[ch["tt"]]; si = 0 if ch["s"]["name"] == "A" else 1; dvw = ch["dvw"]
                p.op("gpsimd", lambda e, ch=ch, sc=sc, si=si, dvw=dvw: e.tensor_scalar(out=ch["R0"][:, 0:dvw], in0=ch["vt"][:], scalar1=sc["beta"][:, si:si + 1], scalar2=None, op0=ALU.mult),
                     reads=[ch["vt"], sc["beta"]], writes=[ch["R0"]])
                p.op("gpsimd", lambda e, ch=ch, sc=sc, si=si, dvw=dvw: e.tensor_scalar(out=ch["R0"][:, dvw:dvw + 128], in0=ch["kn"][:], scalar1=sc["bege"][:, si:si + 1], scalar2=None, op0=ALU.mult),
                     reads=[ch["kn"], sc["bege"], ch["R0"]], writes=[ch["R0"]])
                p.op("gpsimd", lambda e, ch=ch, sc=sc, si=si: e.tensor_scalar(out=ch["kt"][:], in0=ch["kn"][:], scalar1=sc["etail"][:, si:si + 1], scalar2=None, op0=ALU.mult),
                     reads=[ch["kn"], sc["etail"]], writes=[ch["kt"]])
                p.op("gpsimd", lambda e, ch=ch, sc=sc, si=si: e.tensor_scalar(out=ch["qd"][:], in0=ch["qn"][:], scalar1=sc["egc"][:, si:si + 1], scalar2=None, op0=ALU.mult),
                     reads=[ch["qn"], sc["egc"]], writes=[ch["qd"]])
            for ch in chs:
                transp(ch["qd"], ch["qd"][:], 128, ch["qdT"], ch["qdT"][:])
            for lev in range(7):
                a, bnx = lev % 2, (lev + 1) % 2
                for ch in chs:
                    dvw = ch["dvw"]; W = dvw + 128
                    Nk, NTk = ch[f"N{a}"], ch[f"NT{a}"]
                    Rk, Rn = ch[f"R{a}"], ch[f"R{bnx}"]
                    h = HR.next()
                    p.op("tensor", lambda e, h=h, NTk=NTk, Rk=Rk, W=W: e.matmul(h[:, 0:W], lhsT=NTk[:], rhs=Rk[:, 0:W], start=True, stop=True), reads=[NTk, Rk], writes=[h])
                    if lev < 6:
                        p.op("vector", lambda e, h=h, Rk=Rk, Rn=Rn, W=W: e.tensor_tensor(out=Rn[:, 0:W], in0=h[:, 0:W], in1=Rk[:, 0:W], op=ALU.add), reads=[h, Rk], writes=[Rn])
                        qa = QR.next(); qb = QR.next()
                        p.op("tensor", lambda e, qa=qa, Nk=Nk, NTk=NTk: e.matmul(qa[:, 0:128], lhsT=NTk[:], rhs=Nk[:], start=True, stop=True), reads=[Nk, NTk], writes=[qa])
                        p.op("tensor", lambda e, qb=qb, Nk=Nk, NTk=NTk: e.matmul(qb[:, 0:128], lhsT=Nk[:], rhs=NTk[:], start=True, stop=True), reads=[Nk, NTk], writes=[qb])
                        evac(ch[f"N{bnx}"], ch[f"N{bnx}"][:], qa, qa[:, 0:128])
                        evac(ch[f"NT{bnx}"], ch[f"NT{bnx}"][:], qb, qb[:, 0:128])
                    else:
                        p.op("vector", lambda e, h=h, Rk=Rk, ch=ch, dvw=dvw: e.tensor_tensor(out=ch["u"][:], in0=h[:, 0:dvw], in1=Rk[:, 0:dvw], op=ALU.add), reads=[h, Rk], writes=[ch["u"]])
                        p.op("vector", lambda e, h=h, Rk=Rk, ch=ch, dvw=dvw: e.tensor_tensor(out=ch["w"][:], in0=h[:, dvw:dvw + 128], in1=Rk[:, dvw:dvw + 128], op=ALU.add), reads=[h, Rk], writes=[ch["w"]])
            for ch in chs:
                transp(ch["w"], ch["w"][:], 128, ch["wT"], ch["wT"][:])
            for nm in ("gq", "gk"):
                bank, M = fm_proj(blk, nm)
                dst = G["q"] if nm == "gq" else G["k"]
                evac(dst, dst[:], bank, bank[0:64, 2:258])
            lrn = "lrf" if dirn == 0 else "lrb"
            bank, M = fm_proj(blk, lrn)
            lrd = G[f"lr{dirn}"]
            evac(lrd, lrd[:], bank, bank[0:16, 2:258])
            bank = PR.next()
            p.op("tensor", lambda e, bank=bank, lrd=lrd: e.matmul(bank[0:64, 0:256], lhsT=up_sb[:, 64 * dirn:64 * dirn + 64], rhs=lrd[:], start=True, stop=True), reads=[up_sb, lrd], writes=[bank])
            gb = prm_sb[0:64, 40 + dirn:41 + dirn]
            p.op("vector", lambda e, bank=bank: e.tensor_scalar(out=G["zb"][:], in0=bank[0:64, 0:256], scalar1=gb, scalar2=None, op0=ALU.add), reads=[bank, prm_sb], writes=[G["zb"]])
            p.op("scalar", lambda e: e.activation(out=G["ta"][:], in_=G["zb"][:], func=AF.Abs), reads=[G["zb"]], writes=[G["ta"]])
            p.op("scalar", lambda e: e.activation(out=G["ex"][:], in_=G["ta"][:], func=AF.Exp, scale=-1.0), reads=[G["ta"]], writes=[G["ex"]])
            p.op("scalar", lambda e: e.activation(out=G["ta"][:], in_=G["ex"][:], func=AF.Ln, bias=1.0), reads=[G["ex"]], writes=[G["ta"]])
            p.op("vector", lambda e: e.scalar_tensor_tensor(out=G["gkk"][:], in0=G["zb"][:], scalar=0.0, in1=G["ta"][:], op0=ALU.min, op1=ALU.subtract), reads=[G["zb"], G["ta"]], writes=[G["gkk"]])
            p.op("vector", lambda e: e.tensor_scalar(out=G["gkk"][:], in0=G["gkk"][:], scalar1=1.0 / 16.0, scalar2=None, op0=ALU.mult), reads=[G["gkk"]], writes=[G["gkk"]])
            for tt in (0, 1):
                sl = slice(128 * tt, 128 * tt + 128)
                p.op("vector", lambda e, sl=sl: e.tensor_tensor_scan(out=G["gc"][:, sl], data0=G["ones"][:, sl], data1=G["gkk"][:, sl], initial=0.0, op0=ALU.mult, op1=ALU.add),
                     reads=[G["ones"], G["gkk"]], writes=[G["gc"]])
                p.op("vector", lambda e, tt=tt: e.tensor_copy(out=G["glc"][:, tt:tt + 1], in_=G["gc"][:, 128 * tt + 127:128 * tt + 128]), reads=[G["gc"]], writes=[G["glc"]])
                if dirn == 1:
                    p.op("vector", lambda e, sl=sl: e.scalar_tensor_tensor(out=G["gc"][:, sl], in0=G["gc"][:, sl], scalar=-1.0, in1=G["gkk"][:, sl], op0=ALU.mult, op1=ALU.add),
                         reads=[G["gc"], G["gkk"]], writes=[G["gc"]])
                    p.op("vector", lambda e, sl=sl, tt=tt: e.tensor_scalar(out=G["gc"][:, sl], in0=G["gc"][:, sl], scalar1=G["glc"][:, tt:tt + 1], scalar2=None, op0=ALU.add),
                         reads=[G["gc"], G["glc"]], writes=[G["gc"]])
            p.op("scalar", lambda e: e.activation(out=G["egl"][:], in_=G["glc"][:], func=AF.Exp), reads=[G["glc"]], writes=[G["egl"]])
            p.op("scalar", lambda e: e.activation(out=G["ex"][:], in_=G["gc"][:], func=AF.Exp), reads=[G["gc"]], writes=[G["ex"]])
            p.op("vector", lambda e: e.scalar_tensor_tensor(out=G["qpos"][:], in0=G["q"][:], scalar=64.0 ** -0.5, in1=G["ex"][:], op0=ALU.mult, op1=ALU.mult), reads=[G["q"], G["ex"]], writes=[G["qpos"]])
            p.op("scalar", lambda e: e.activation(out=G["ex"][:], in_=G["gc"][:], func=AF.Exp, scale=-1.0), reads=[G["gc"], G["qpos"]], writes=[G["ex"]])
            p.op("vector", lambda e: e.tensor_tensor(out=G["kneg"][:], in0=G["k"][:], in1=G["ex"][:], op=ALU.mult), reads=[G["k"], G["ex"]], writes=[G["kneg"]])
            for tt in (0, 1):
                sl = slice(128 * tt, 128 * tt + 128)
                p.op("scalar", lambda e, sl=sl, tt=tt: e.activation(out=G["ta"][:, sl], in_=G["gc"][:, sl], func=AF.Exp, scale=-1.0, bias=G["glc"][:, tt:tt + 1]),
                     reads=[G["gc"], G["glc"]], writes=[G["ta"]])
            p.op("vector", lambda e: e.tensor_tensor(out=G["ktl"][:], in0=G["k"][:], in1=G["ta"][:], op=ALU.mult), reads=[G["k"], G["ta"]], writes=[G["ktl"]])
            for tt in (0, 1):
                sl = slice(128 * tt, 128 * tt + 128)
                gt = GT[tt]
                q = QR.next()
                p.op("tensor", lambda e, q=q, sl=sl: e.matmul(q[:, 0:128], lhsT=G["kneg"][:, sl], rhs=G["qpos"][:, sl], start=True, stop=True), reads=[G["kneg"], G["qpos"]], writes=[q])
                p.op("vector", lambda e, q=q, gt=gt: e.tensor_tensor(out=gt["AT"][:], in0=q[:, 0:128], in1=GM, op=ALU.mult), reads=[q, cst_sb], writes=[gt["AT"]])
                q2 = QR.next()
                p.op("tensor", lambda e, q2=q2, sl=sl: e.matmul(q2[:, 0:64], lhsT=G["ktl"][:, sl], rhs=idb[0:64, 0:64], start=True, stop=True), reads=[G["ktl"], idb], writes=[q2])
                evac(gt["kt"], gt["kt"][:], q2, q2[:, 0:64])
            for tt in tiles:
                t0 = b * 256 + tt * 128
                sc = SC[tt]
                if dirn == 1:
                    p.dma("sync", sc["ofl"][:], ofw_t[t0 // 128][:, :], reads=[ofw_t[t0 // 128]], writes=[sc["ofl"]], sembuf=sc["ofl"])
                tch = [c for c in chs if c["tt"] == tt]
                for ch in tch:
                    s = ch["s"]; dvw = ch["dvw"]
                    q = QR.next(); ch["_q"] = q
                    p.op("tensor", lambda e, q=q, ch=ch, s=s, dvw=dvw: e.matmul(q[:, 0:dvw], lhsT=ch["wT"][:], rhs=s["Sb"][:], start=True, stop=True), reads=[ch["wT"], s["Sb"]], writes=[q])
                gt = GT[tt]; sl = slice(128 * tt, 128 * tt + 128)
                qo = QR.next()
                p.op("tensor", lambda e, qo=qo, sl=sl: e.matmul(qo[:, 0:128], lhsT=G["qpos"][:, sl], rhs=G["Sb"][:], start=True, stop=False), reads=[G["qpos"], G["Sb"]], writes=[qo])
                p.op("tensor", lambda e, qo=qo, gt=gt, sc=sc: e.matmul(qo[:, 0:128], lhsT=gt["AT"][:], rhs=sc["gvb"][:], start=False, stop=True), reads=[gt["AT"], sc["gvb"]], writes=[qo])
                qsg = QR.next()
                p.op("tensor", lambda e, qsg=qsg, gt=gt, sc=sc: e.matmul(qsg[0:64, 0:128], lhsT=gt["kt"][:], rhs=sc["gvb"][:], start=True, stop=True), reads=[gt["kt"], sc["gvb"]], writes=[qsg])
                for ch in tch:
                    dvw = ch["dvw"]; q = ch["_q"]
                    p.op("vector", lambda e, q=q, ch=ch, dvw=dvw: e.tensor_tensor(out=ch["vn"][:], in0=ch["u"][:], in1=q[:, 0:dvw], op=ALU.subtract), reads=[ch["u"], q], writes=[ch["vn"]])
                p.op("vector", lambda e, qsg=qsg, tt=tt: e.scalar_tensor_tensor(out=G["S"][:], in0=G["S"][:], scalar=G["egl"][:, tt:tt + 1], in1=qsg[0:64, 0:128], op0=ALU.mult, op1=ALU.add),
                     reads=[G["S"], G["egl"], qsg], writes=[G["S"]])
                p.op("scalar", lambda e: e.copy(out=G["Sb"][:], in_=G["S"][:]), reads=[G["S"]], writes=[G["Sb"]])
                for ch in tch:
                    s = ch["s"]; dvw = ch["dvw"]
                    qo_ = QR.next(); ch["_qo"] = qo_
                    p.op("tensor", lambda e, qo_=qo_, ch=ch, s=s, dvw=dvw: e.matmul(qo_[:, 0:dvw], lhsT=ch["qdT"][:], rhs=s["Sb"][:], start=True, stop=False), reads=[ch["qdT"], s["Sb"]], writes=[qo_])
                    p.op("tensor", lambda e, qo_=qo_, ch=ch, dvw=dvw: e.matmul(qo_[:, 0:dvw], lhsT=ch["qkDT"][:], rhs=ch["vn"][:], start=False, stop=True), reads=[ch["qkDT"], ch["vn"]], writes=[qo_])
                    qs = QR.next(); ch["_qs"] = qs
                    p.op("tensor", lambda e, qs=qs, ch=ch, dvw=dvw: e.matmul(qs[:, 0:dvw], lhsT=ch["kt"][:], rhs=ch["vn"][:], start=True, stop=True), reads=[ch["kt"], ch["vn"]], writes=[qs])
                for ch in tch:
                    s = ch["s"]; dvw = ch["dvw"]; si = 0 if s["name"] == "A" else 1; qs = ch["_qs"]; qo_ = ch["_qo"]
                    p.op("vector", lambda e, qs=qs, s=s, sc=sc, si=si, dvw=dvw: e.scalar_tensor_tensor(out=s["S"][:], in0=s["S"][:], scalar=sc["egl"][:, si:si + 1], in1=qs[:, 0:dvw], op0=ALU.mult, op1=ALU.add),
                         reads=[s["S"], sc["egl"], qs], writes=[s["S"]])
                    p.op("scalar", lambda e, s=s: e.copy(out=s["Sb"][:], in_=s["S"][:]), reads=[s["S"]], writes=[s["Sb"]])
                    oc = s["oc"]
                    if dirn == 0:
                        p.op("scalar", lambda e, ch=ch, qo_=qo_, dvw=dvw: e.copy(out=ch["osb"][:], in_=qo_[:, 0:dvw]), reads=[qo_], writes=[ch["osb"]])
                        p.dma("sync", ofw_t[t0 // 128][:, oc:oc + dvw], ch["osb"][:], reads=[ch["osb"]], writes=[ofw_t[t0 // 128]], sembuf=ch["osb"])
                    else:
                        p.op("vector", lambda e, ch=ch, qo_=qo_, dvw=dvw, sc=sc, oc=oc: e.tensor_tensor(out=ch["osb"][:], in0=qo_[:, 0:dvw], in1=sc["ofl"][:, oc:oc + dvw], op=ALU.add), reads=[qo_, sc["ofl"]], writes=[ch["osb"]])
                        dst = oA if s["name"] == "A" else oB
                        p.dma("sync", dst[t0:t0 + 128, :], ch["osb"][:], reads=[ch["osb"]], sembuf=ch["osb"])
                if dirn == 0:
                    p.op("scalar", lambda e, gt=gt, qo=qo: e.copy(out=gt["osb"][:], in_=qo[:, 0:128]), reads=[qo], writes=[gt["osb"]])
                    p.dma("sync", ofw_t[t0 // 128][:, 192:320], gt["osb"][:], reads=[gt["osb"]], writes=[ofw_t[t0 // 128]], sembuf=gt["osb"])
                else:
                    p.op("vector", lambda e, gt=gt, qo=qo, sc=sc: e.tensor_tensor(out=gt["osb"][:], in0=qo[:, 0:128], in1=sc["ofl"][:, 192:320], op=ALU.add), reads=[qo, sc["ofl"]], writes=[gt["osb"]])
                    p.dma("sync", ogl[t0:t0 + 128, :], gt["osb"][:], reads=[gt["osb"]], sembuf=gt["osb"])


def build_att(S, xdt=BF16):
    p = PB(); nc = p.nc
    NT = S // 128; NB = S // 256; NQB = NB // 2; SQ = S // 2
    ext = lambda n, s, d: nc.dram_tensor(n, s, d, kind="ExternalInput").ap()
    xb = ext("xb", [NB, 128, 16 * 260], xdt)
    xq = ext("xq", [NQB, 128, 16 * 260], xdt)
    wfa = ext("wfa", [128, 16 * 512], F32)
    wta = ext("wta", [128, 16 * 128], F32)
    prm = ext("prm", [128, 64], F32)
    cst = ext("cst", [128, len(CN) * 128], F32)
    cosk = ext("cosk", [128, S], F32); sink = ext("sink", [128, S], F32)
    cosq = ext("cosq", [128, SQ], F32); sinq = ext("sinq", [128, SQ], F32)
    oat = nc.dram_tensor("oat", [SQ, 384], F32, kind="ExternalOutput").ap()
    phase_att(p, S, xdt, None, xb, xq, wfa, wta, prm, cst, cosk, sink, cosq, sinq, oat)
    p.finish()
    print("att instr", p.ninstr, "sems", len(p.sems))
    return p.emit()


def phase_att(p, S, xdt, banks, xb, xq, wfa, wta, prm, cst, cosk, sink, cosq, sinq, oat):
    nc = p.nc
    NT = S // 128; NB = S // 256; NQB = NB // 2; SQ = S // 2

    wfa_sb = p.sb("wfa_sb", [128, 16, 512], BF16)
    wta_sb = p.sb("wta_sb", [128, 16, 128], BF16)
    prm_sb = p.sb("prm_sb_a", [128, 64], F32)
    cst_sb = p.sb("cst_sb_a", [128, len(CN) * 128], F32)
    C = {n: cst_sb[:, i * 128:(i + 1) * 128] for i, n in enumerate(CN)}
    onesb = p.sb("onesb", [128, 128], BF16)
    for k in range(16):
        p.dma("gpsimd", wfa_sb[:, k, :], wfa[:, k * 512:(k + 1) * 512], writes=[wfa_sb])
        p.dma("gpsimd", wta_sb[:, k, :], wta[:, k * 128:(k + 1) * 128], writes=[wta_sb])
    p.dma("sync", prm_sb[:], prm[:, :], writes=[prm_sb])
    p.dma("sync", cst_sb[:], cst[:, :], writes=[cst_sb])
    p.op("vector", lambda e: e.tensor_copy(out=onesb[:], in_=C["ONES"]), reads=[cst_sb], writes=[onesb])

    KT = p.sb("KT", [128, S], BF16)
    V = p.sb("V", [128, NT, 132], BF16)
    QT = [p.sb(f"QT{h}", [128, SQ], BF16) for h in range(3)]
    p.op("gpsimd", lambda e: e.memset(V[:, :, 128:129], 1.0), writes=[V])
    xblk = [p.sb(f"xblka{i}", [128, 16, 260], BF16) for i in range(2)]
    cosb = [p.sb(f"cosb{i}", [128, 256], F32) for i in range(2)]
    sinb = [p.sb(f"sinb{i}", [128, 256], F32) for i in range(2)]
    W = {}
    for i in range(2):
        for nm, dt in [("sq", BF16), ("rs", F32), ("kn", F32), ("t1", F32), ("t2", F32)]:
            W[nm, i] = p.sb(f"w_{nm}{i}", [128, 256], dt)
    if banks is None:
        banks = [p.ps(f"bank{i}", [128, 512], F32) for i in range(8)]
    PR = Ring(banks[0:4])
    wi = [0]

    def normrope(bank, gcol, cb, sb_, dst_buf, dst_ap):
        i = wi[0] % 2; wi[0] += 1
        sq, rs, kn, t1, t2 = (W[n, i] for n in ("sq", "rs", "kn", "t1", "t2"))
        raw = bank[:, 2:258]
        p.op("scalar", lambda e: e.activation(out=sq[:], in_=raw, func=AF.Square), reads=[bank], writes=[sq])
        b2 = PR.next()
        p.op("tensor", lambda e: e.matmul(b2[:, 0:256], lhsT=onesb[:], rhs=sq[:], start=True, stop=True), reads=[onesb, sq], writes=[b2])
        p.op("scalar", lambda e: e.activation(out=rs[:], in_=b2[:, 0:256], func=AF.Sqrt, scale=1.0 / 128.0, bias=EPS), reads=[b2], writes=[rs])
        p.op("vector", lambda e: e.reciprocal(out=rs[:], in_=rs[:]), reads=[rs], writes=[rs])
        p.op("vector", lambda e: e.scalar_tensor_tensor(out=kn[:], in0=raw, scalar=gcol, in1=rs[:], op0=ALU.mult, op1=ALU.mult), reads=[bank, prm_sb, rs], writes=[kn])
        b3 = PR.next()
        p.op("tensor", lambda e: e.matmul(b3[:, 0:256], lhsT=C["ROT"], rhs=kn[:], start=True, stop=True), reads=[cst_sb, kn], writes=[b3])
        p.op("gpsimd", lambda e: e.tensor_tensor(out=t1[:], in0=kn[:], in1=cb[:], op=ALU.mult), reads=[kn, cb], writes=[t1])
        p.op("vector", lambda e: e.tensor_tensor(out=t2[:], in0=b3[:, 0:256], in1=sb_[:], op=ALU.mult), reads=[b3, sb_], writes=[t2])
        p.op("vector", lambda e: e.tensor_tensor(out=dst_ap, in0=t1[:], in1=t2[:], op=ALU.add), reads=[t1, t2], writes=[dst_buf])

    def fm_proj(blk, m0):
        bank = PR.next()
        for k in range(16):
            p.op("tensor", lambda e, k=k: e.matmul(bank[:, 0:260], lhsT=wfa_sb[:, k, m0:m0 + 128], rhs=blk[:, k, :], start=(k == 0), stop=(k == 15)),
                 reads=[wfa_sb, blk], writes=[bank], inc=(k == 15))
        return bank

    def load_blk(i, src, b, ctab, stab):
        blk = xblk[i % 2]
        if xdt == BF16:
            p.dma("sync", blk[:].rearrange("p k t -> p (k t)"), src[b, :, :], writes=[blk])
        else:
            for k in range(16):
                p.dma("gpsimd", blk[:, k, :], src[b, :, k * 260:(k + 1) * 260], writes=[blk])
        cb = cosb[i % 2]; sb_ = sinb[i % 2]
        p.dma("sync", cb[:], ctab[:, b * 256:(b + 1) * 256], writes=[cb])
        p.dma("sync", sb_[:], stab[:, b * 256:(b + 1) * 256], writes=[sb_])
        return blk, cb, sb_

    for b in range(NB):
        blk, cb, sb_ = load_blk(b, xb, b, cosk, sink)
        bank = fm_proj(blk, 384)
        normrope(bank, prm_sb[:, 39:40], cb, sb_, KT, KT[:, b * 256:(b + 1) * 256])
        for tt in range(2):
            bank = PR.next()
            for k in range(16):
                p.op("tensor", lambda e, k=k, tt=tt, bank=bank: e.matmul(bank[:, 0:128], lhsT=blk[:, k, 2 + 128 * tt:2 + 128 * (tt + 1)], rhs=wta_sb[:, k, :], start=(k == 0), stop=(k == 15)),
                     reads=[blk, wta_sb], writes=[bank], inc=(k == 15))
            p.op("scalar", lambda e, bank=bank, tt=tt: e.copy(out=V[:, 2 * b + tt, 0:128], in_=bank[:, 0:128]), reads=[bank], writes=[V])
    for b in range(NQB):
        blk, cb, sb_ = load_blk(NB + b, xq, b, cosq, sinq)
        for h in range(3):
            bank = fm_proj(blk, 128 * h)
            normrope(bank, prm_sb[:, 38:39], cb, sb_, QT[h], QT[h][:, b * 256:(b + 1) * 256])
    pT = [p.sb(f"pT{i}", [128, 512], BF16) for i in range(3)]
    rec = [p.sb(f"rec{i}", [128, 1], F32) for i in range(4)]
    osb = [p.sb(f"osba{i}", [128, 128], F32) for i in range(4)]
    PSR = Ring(banks[0:4])
    po = banks[4:8]
    scale = 128.0 ** -0.5
    it = 0
    QW = min(512, SQ)
    NQS = QW // 128
    for h in range(3):
        for qb in range(SQ // QW):
            for kt in range(NT):
                ps = PSR.next()
                p.op("tensor", lambda e, ps=ps, kt=kt, h=h, qb=qb: e.matmul(ps[:, 0:QW], lhsT=KT[:, kt * 128:(kt + 1) * 128], rhs=QT[h][:, qb * QW:(qb + 1) * QW], start=True, stop=True),
                     reads=[KT, QT[h]], writes=[ps])
                pt = pT[it % 3]; it += 1
                p.op("scalar", lambda e, ps=ps, pt=pt: e.activation(out=pt[:, 0:QW], in_=ps[:, 0:QW], func=AF.Exp, scale=scale), reads=[ps], writes=[pt])
                for qs in range(NQS):
                    p.op("tensor", lambda e, qs=qs, pt=pt, kt=kt: e.matmul(po[qs][:, 0:129], lhsT=pt[:, qs * 128:(qs + 1) * 128], rhs=V[:, kt, 0:129], start=(kt == 0), stop=(kt == NT - 1)),
                         reads=[pt, V], writes=[po[qs]], inc=(kt == NT - 1))
            for qs in range(NQS):
                p.op("vector", lambda e, qs=qs: e.reciprocal(out=rec[qs][:], in_=po[qs][:, 128:129]), reads=[po[qs]], writes=[rec[qs]])
                p.op("scalar", lambda e, qs=qs: e.activation(out=osb[qs][:], in_=po[qs][:, 0:128], func=AF.Copy, scale=rec[qs][:]), reads=[po[qs], rec[qs]], writes=[osb[qs]])
                q0 = qb * QW + qs * 128
                p.dma("sync", oat[q0:q0 + 128, h * 128:(h + 1) * 128], osb[qs][:], reads=[osb[qs]], sembuf=osb[qs])


ALPHA = (2.0 * 4) ** 0.25


def layernorm_tile(p, z, out, lng, lnb, st, mv, tmp):
    for c in range(4):
        p.op("vector", lambda e, c=c: e.bn_stats(out=st[:, c, :], in_=z[:, c * 512:(c + 1) * 512]), reads=[z], writes=[st])
    p.op("vector", lambda e: e.bn_aggr(out=mv[:, 0:2], in_=st[:].rearrange("p a b -> p (a b)")), reads=[st], writes=[mv])
    p.op("scalar", lambda e: e.activation(out=mv[:, 2:3], in_=mv[:, 1:2], func=AF.Sqrt, bias=EPS, scale=1.0), reads=[mv], writes=[mv])
    p.op("vector", lambda e: e.reciprocal(out=mv[:, 3:4], in_=mv[:, 2:3]), reads=[mv], writes=[mv])
    p.op("vector", lambda e: e.tensor_scalar(out=tmp[:], in0=z[:], scalar1=mv[:, 0:1], scalar2=mv[:, 3:4], op0=ALU.subtract, op1=ALU.mult), reads=[z, mv], writes=[tmp])
    p.op("gpsimd", lambda e: e.tensor_tensor(out=tmp[:], in0=tmp[:], in1=lng[:], op=ALU.mult), reads=[tmp, lng], writes=[tmp])
    p.op("vector", lambda e: e.tensor_tensor(out=out[:], in0=tmp[:], in1=lnb[:], op=ALU.add), reads=[tmp, lnb], writes=[out])


def transpose_out(p, src_f32, banks4, idf, xTf, xTb):
    for g in range(4):
        bank = banks4[g]
        for j in range(4):
            c = 4 * g + j
            p.op("tensor", lambda e, c=c, j=j, bank=bank: e.matmul(bank[:, j * 128:(j + 1) * 128], lhsT=src_f32[:, c * 128:(c + 1) * 128], rhs=idf[:], start=True, stop=True),
                 reads=[src_f32, idf], writes=[bank])
        p.op("scalar", lambda e, g=g, bank=bank: e.copy(out=xTf[:, 4 * g:4 * g + 4, :].rearrange("p a b -> p (a b)"), in_=bank[:, :]), reads=[bank], writes=[xTf])
    p.op("gpsimd", lambda e: e.tensor_copy(out=xTb[:], in_=xTf[:]), reads=[xTf], writes=[xTb])


def build_post(TT, moe):
    p = PB(); nc = p.nc
    T = TT * 128
    ext = lambda n, s, d: nc.dram_tensor(n, s, d, kind="ExternalInput").ap()
    out = lambda n, s, d: nc.dram_tensor(n, s, d, kind="ExternalOutput").ap()
    o_in = ext("o", [T, 2048], F32); gt_in = ext("gt", [T, 1280], F32); x_in = ext("x", [T, 2048], F32)
    wout = ext("wout", [128, 16 * 2048], F32)
    nrm = ext("nrm", [128, 256], F32)
    ln = ext("ln", [128, 4096], F32)
    ident = ext("ident", [128, 128], F32)
    x1_out = out("x1", [T, 2048], F32); x1T_out = out("x1T", [TT, 128, 16 * 128], BF16)
    if moe:
        rw = ext("rw", [128, 16 * 8], F32)
        g_out = out("gates", [T, 8], F32)
    wout_sb = p.sb("wout_sb", [128, 16, 2048], BF16)
    for k in range(16):
        p.dma("gpsimd", wout_sb[:, k, :], wout[:, k * 2048:(k + 1) * 2048], writes=[wout_sb])
    nrm_sb = p.sb("nrm_sb", [128, 256], F32); lng = p.sb("lng", [128, 2048], F32); lnb = p.sb("lnb", [128, 2048], F32)
    idf = p.sb("idf", [128, 128], F32); idb = p.sb("idb", [128, 128], BF16)
    p.dma("sync", nrm_sb[:], nrm[:, :], writes=[nrm_sb])
    p.dma("sync", lng[:], ln[:, 0:2048], writes=[lng]); p.dma("sync", lnb[:], ln[:, 2048:4096], writes=[lnb])
    p.dma("sync", idf[:], ident[:, :], writes=[idf])
    p.op("vector", lambda e: e.tensor_copy(out=idb[:], in_=idf[:]), reads=[idf], writes=[idb])
    if moe:
        rw_sb = p.sb("rw_sb", [128, 16, 8], F32)
        p.dma("sync", rw_sb[:].rearrange("p a b -> p (a b)"), rw[:, :], writes=[rw_sb])
    banks = [p.ps(f"bank{i}", [128, 512], F32) for i in range(8)]
    ob = [p.sb(f"ob{i}", [128, 2048], F32) for i in range(2)]
    gb = [p.sb(f"gb{i}", [128, 1280], F32) for i in range(2)]
    xbf = [p.sb(f"xb{i}", [128, 2048], F32) for i in range(2)]
    sq = p.sb("sq", [128, 1280], F32); ss = p.sb("ss", [128, 32], F32); sg = p.sb("sg", [128, 1280], F32)
    mix = p.sb("mix", [128, 2048], BF16); mixT = p.sb("mixT", [128, 16, 128], BF16)
    z = p.sb("z", [128, 2048], F32); x1 = p.sb("x1s", [128, 2048], F32); tmp = p.sb("tmp", [128, 2048], F32)
    st = p.sb("st", [128, 4, 6], F32); mv = p.sb("mv", [128, 4], F32)
    xTf = p.sb("xTf", [128, 16, 128], F32); xTb = p.sb("xTb", [128, 16, 128], BF16)
    if moe:
        lg = p.sb("lg", [128, 8], F32); mx = p.sb("mx", [128, 8], F32); gs = p.sb("gs", [128, 8], F32); ga = p.sb("ga", [128, 8], F32); gbb = p.sb("gbb", [128, 8], F32)
    for t in range(TT):
        o = ob[t % 2]; g = gb[t % 2]; x = xbf[t % 2]
        r0 = t * 128
        p.dma("sync", o[:], o_in[r0:r0 + 128, :], writes=[o])
        p.dma("sync", g[:], gt_in[r0:r0 + 128, :], writes=[g])
        p.dma("sync", x[:], x_in[r0:r0 + 128, :], writes=[x])
        p.op("vector", lambda e, o=o: e.tensor_tensor(out=sq[:, 0:768], in0=o[:, 0:768], in1=o[:, 0:768], op=ALU.mult), reads=[o], writes=[sq])
        p.op("gpsimd", lambda e, o=o: e.tensor_tensor(out=sq[:, 768:1280], in0=o[:, 1536:2048], in1=o[:, 1536:2048], op=ALU.mult), reads=[o], writes=[sq])
        p.op("vector", lambda e: e.tensor_reduce(out=ss[:, 0:10], in_=sq[:].rearrange("p (h d) -> p h d", d=128), axis=AX.X, op=ALU.add), reads=[sq], writes=[ss])
        p.op("scalar", lambda e: e.activation(out=ss[:, 10:20], in_=ss[:, 0:10], func=AF.Sqrt, scale=1.0 / 128.0, bias=EPS), reads=[ss], writes=[ss])
        p.op("vector", lambda e: e.reciprocal(out=ss[:, 20:30], in_=ss[:, 10:20]), reads=[ss], writes=[ss])
        p.op("scalar", lambda e, g=g: e.activation(out=sg[:], in_=g[:], func=AF.Silu), reads=[g], writes=[sg])
        for h in range(10):
            oc = h * 128 if h < 6 else 1536 + (h - 6) * 128
            gcol = nrm_sb[:, 0:128] if h < 6 else nrm_sb[:, 128:256]
            p.op("vector", lambda e, h=h, oc=oc, gcol=gcol, o=o: e.scalar_tensor_tensor(out=sq[:, h * 128:(h + 1) * 128], in0=o[:, oc:oc + 128], scalar=ss[:, 20 + h:21 + h], in1=gcol, op0=ALU.mult, op1=ALU.mult),
                 reads=[o, ss, nrm_sb, sq], writes=[sq])
        p.op("vector", lambda e: e.tensor_tensor(out=mix[:, 0:768], in0=sq[:, 0:768], in1=sg[:, 0:768], op=ALU.mult), reads=[sq, sg], writes=[mix])
        p.op("gpsimd", lambda e: e.tensor_tensor(out=mix[:, 1536:2048], in0=sq[:, 768:1280], in1=sg[:, 768:1280], op=ALU.mult), reads=[sq, sg, mix], writes=[mix])
        p.op("gpsimd", lambda e, o=o: e.tensor_copy(out=mix[:, 768:1536], in_=o[:, 768:1536]), reads=[o, mix], writes=[mix])
        for gq in range(4):
            bank = banks[gq]
            for j in range(4):
                c = 4 * gq + j
                p.op("tensor", lambda e, c=c, j=j, bank=bank: e.matmul(bank[:, j * 128:(j + 1) * 128], lhsT=mix[:, c * 128:(c + 1) * 128], rhs=idb[:], start=True, stop=True),
                     reads=[mix, idb], writes=[bank])
            p.op("scalar", lambda e, gq=gq, bank=bank: e.copy(out=mixT[:, 4 * gq:4 * gq + 4, :].rearrange("p a b -> p (a b)"), in_=bank[:, :]), reads=[bank], writes=[mixT])
        for nb in range(4):
            bank = banks[4 + nb]
            for k in range(16):
                p.op("tensor", lambda e, k=k, nb=nb, bank=bank: e.matmul(bank[:, :], lhsT=mixT[:, k, :], rhs=wout_sb[:, k, nb * 512:(nb + 1) * 512], start=(k == 0), stop=(k == 15)),
                     reads=[mixT, wout_sb], writes=[bank], inc=(k == 15))
            p.op("vector", lambda e, nb=nb, bank=bank, x=x: e.scalar_tensor_tensor(out=z[:, nb * 512:(nb + 1) * 512], in0=x[:, nb * 512:(nb + 1) * 512], scalar=ALPHA, in1=bank[:, :], op0=ALU.mult, op1=ALU.add),
                 reads=[x, bank, z], writes=[z])
        layernorm_tile(p, z, x1, lng, lnb, st, mv, tmp)
        p.dma("sync", x1_out[r0:r0 + 128, :], x1[:], reads=[x1], sembuf=x1)
        transpose_out(p, x1, banks[0:4], idf, xTf, xTb)
        p.dma("sync", x1T_out[t, :, :], xTb[:].rearrange("p a b -> p (a b)"), reads=[xTb], sembuf=xTb)
        if moe:
            bank = banks[4]
            for k in range(16):
                p.op("tensor", lambda e, k=k: e.matmul(bank[:, 0:8], lhsT=xTf[:, k, :], rhs=rw_sb[:, k, :], start=(k == 0), stop=(k == 15)), reads=[xTf, rw_sb], writes=[bank], inc=(k == 15))
            p.op("scalar", lambda e: e.copy(out=lg[:], in_=bank[:, 0:8]), reads=[bank], writes=[lg])
            p.op("vector", lambda e: e.max(out=mx[:], in_=lg[:]), reads=[lg], writes=[mx])
            p.op("vector", lambda e: e.tensor_tensor(out=gs[:, 0:1], in0=mx[:, 1:2], in1=mx[:, 0:1], op=ALU.subtract), reads=[mx], writes=[gs])
            p.op("scalar", lambda e: e.activation(out=gs[:, 1:2], in_=gs[:, 0:1], func=AF.Exp), reads=[gs], writes=[gs])
            p.op("vector", lambda e: e.tensor_scalar(out=gs[:, 2:3], in0=gs[:, 1:2], scalar1=1.0, scalar2=None, op0=ALU.add), reads=[gs], writes=[gs])
            p.op("vector", lambda e: e.reciprocal(out=gs[:, 3:4], in_=gs[:, 2:3]), reads=[gs], writes=[gs])
            p.op("vector", lambda e: e.tensor_tensor(out=gs[:, 4:5], in0=gs[:, 1:2], in1=gs[:, 3:4], op=ALU.mult), reads=[gs], writes=[gs])
            p.op("vector", lambda e: e.tensor_scalar(out=ga[:], in0=lg[:], scalar1=mx[:, 0:1], scalar2=gs[:, 3:4], op0=ALU.is_equal, op1=ALU.mult), reads=[lg, mx, gs], writes=[ga])
            p.op("vector", lambda e: e.tensor_scalar(out=gbb[:], in0=lg[:], scalar1=mx[:, 1:2], scalar2=gs[:, 4:5], op0=ALU.is_equal, op1=ALU.mult), reads=[lg, mx, gs], writes=[gbb])
            p.op("vector", lambda e: e.tensor_tensor(out=ga[:], in0=ga[:], in1=gbb[:], op=ALU.add), reads=[ga, gbb], writes=[ga])
            p.dma("sync", g_out[r0:r0 + 128, :], ga[:], reads=[ga], sembuf=ga)
    p.finish()
    print("post instr", p.ninstr, "sems", len(p.sems))
    return p.emit()


def build_combine(TT, moe, NE=8):
    p = PB(); nc = p.nc
    T = TT * 128
    ext = lambda n, s, d: nc.dram_tensor(n, s, d, kind="ExternalInput").ap()
    out = lambda n, s, d: nc.dram_tensor(n, s, d, kind="ExternalOutput").ap()
    x1_in = ext("x1", [T, 2048], F32); y_in = ext("y", [NE, T, 2048], BF16)
    ln = ext("ln", [128, 4096], F32); ident = ext("ident", [128, 128], F32)
    if moe:
        g_in = ext("gates", [T, 8], F32)
    x2_out = out("x2", [T, 2048], F32); x2T_out = out("x2T", [TT, 128, 16 * 128], BF16)
    lng = p.sb("lng", [128, 2048], F32); lnb = p.sb("lnb", [128, 2048], F32); idf = p.sb("idf", [128, 128], F32)
    p.dma("sync", lng[:], ln[:, 0:2048], writes=[lng]); p.dma("sync", lnb[:], ln[:, 2048:4096], writes=[lnb]); p.dma("sync", idf[:], ident[:, :], writes=[idf])
    banks = [p.ps(f"bank{i}", [128, 512], F32) for i in range(4)]
    xb_ = [p.sb(f"x1b{i}", [128, 2048], F32) for i in range(2)]
    yb = [p.sb(f"yb{i}", [128, 2048], BF16) for i in range(3)]
    gsb = [p.sb(f"gsb{i}", [128, 8], F32) for i in range(2)]
    acc = p.sb("acc", [128, 2048], F32); x2 = p.sb("x2s", [128, 2048], F32); tmp = p.sb("tmp", [128, 2048], F32)
    st = p.sb("st", [128, 4, 6], F32); mv = p.sb("mv", [128, 4], F32)
    xTf = p.sb("xTf", [128, 16, 128], F32); xTb = p.sb("xTb", [128, 16, 128], BF16)
    yi = 0
    for t in range(TT):
        r0 = t * 128
        x1 = xb_[t % 2]
        p.dma("sync", x1[:], x1_in[r0:r0 + 128, :], writes=[x1])
        if moe:
            gt = gsb[t % 2]
            p.dma("sync", gt[:], g_in[r0:r0 + 128, :], writes=[gt])
        p.op("vector", lambda e, x1=x1: e.tensor_scalar(out=acc[:], in0=x1[:], scalar1=ALPHA, scalar2=None, op0=ALU.mult), reads=[x1, acc], writes=[acc])
        for ei in range(NE):
            y = yb[yi % 3]; yi += 1
            p.dma("sync" if ei % 2 == 0 else "scalar", y[:], y_in[ei, r0:r0 + 128, :], writes=[y])
            if moe:
                p.op("vector", lambda e, y=y, ei=ei, gt=gt: e.scalar_tensor_tensor(out=acc[:], in0=y[:], scalar=gt[:, ei:ei + 1], in1=acc[:], op0=ALU.mult, op1=ALU.add), reads=[y, gt, acc], writes=[acc])
            else:
                p.op("vector", lambda e, y=y: e.tensor_tensor(out=acc[:], in0=y[:], in1=acc[:], op=ALU.add), reads=[y, acc], writes=[acc])
        layernorm_tile(p, acc, x2, lng, lnb, st, mv, tmp)
        p.dma("sync", x2_out[r0:r0 + 128, :], x2[:], reads=[x2], sembuf=x2)
        transpose_out(p, x2, banks, idf, xTf, xTb)
        p.dma("sync", x2T_out[t, :, :], xTb[:].rearrange("p a b -> p (a b)"), reads=[xTb], sembuf=xTb)
    p.finish()
    print("combine instr", p.ninstr, "sems", len(p.sems))
    return p.emit()


def build_ffn(NP, F, gc, TP=1024):
    p = PB(); nc = p.nc
    NCH = F // 128; NG = NCH // gc; GW = gc * 128
    NTB = TP // 512; NTT = TP // 128
    ext = lambda n, s, d: nc.dram_tensor(n, s, d, kind="ExternalInput").ap()
    xT = ext("xT", [NP, 128, 16 * TP], BF16)
    wg = ext("wg", [128, 16 * F], F32); wu = ext("wu", [128, 16 * F], F32); wd = ext("wd", [128, NCH * 2048], F32)
    y_out = nc.dram_tensor("y", [NP * TP, 2048], BF16, kind="ExternalOutput").ap()
    banks = [p.ps(f"bank{i}", [128, 512], F32) for i in range(8)]
    HB = Ring(banks[0:4]); YB = Ring(banks[4:8])
    xs = [p.sb(f"xs{i}", [128, 16, TP], BF16) for i in range(1)]
    wgs = [p.sb(f"wgs{i}", [128, 16, GW], BF16) for i in range(2)]
    wus = [p.sb(f"wus{i}", [128, 16, GW], BF16) for i in range(2)]
    wds = [p.sb(f"wds{i}", [128, gc, 2048], BF16) for i in range(2)]
    hT = [p.sb(f"hT{i}", [128, gc, TP], BF16) for i in range(2)]
    sgb = [p.sb(f"sgb{i}", [128, 512], F32) for i in range(2)]
    yacc = p.sb("yacc", [128, NTT, 2048], F32)
    yob = [p.sb(f"yob{i}", [128, 2048], BF16) for i in range(2)]
    gi = 0; si = 0
    for ps_ in range(NP):
        x = xs[0]
        p.dma("sync", x[:].rearrange("p k t -> p (k t)"), xT[ps_, :, :], writes=[x])
        for g in range(NG):
            wg_s = wgs[gi % 2]; wu_s = wus[gi % 2]; wd_s = wds[gi % 2]; h = hT[gi % 2]; gi += 1
            f0 = g * GW
            wg3 = wg.rearrange("p (k f) -> p k f", k=16); wu3 = wu.rearrange("p (k f) -> p k f", k=16)
            for kh in range(2):
                p.dma("gpsimd", wg_s[:, 8 * kh:8 * kh + 8, :], wg3[:, 8 * kh:8 * kh + 8, f0:f0 + GW], writes=[wg_s])
                p.dma("gpsimd", wu_s[:, 8 * kh:8 * kh + 8, :], wu3[:, 8 * kh:8 * kh + 8, f0:f0 + GW], writes=[wu_s])
            p.dma("gpsimd", wd_s[:].rearrange("p c d -> p (c d)"), wd[:, g * gc * 2048:(g + 1) * gc * 2048], writes=[wd_s], max_dma_last_dim=8192)
            for c in range(gc):
                for tb in range(NTB):
                    pg = HB.next(); pu = HB.next()
                    for k in range(16):
                        p.op("tensor", lambda e, k=k, c=c, tb=tb, pg=pg: e.matmul(pg[:, :], lhsT=wg_s[:, k, c * 128:(c + 1) * 128], rhs=x[:, k, tb * 512:(tb + 1) * 512], start=(k == 0), stop=(k == 15)), reads=[wg_s, x], writes=[pg], inc=(k == 15))
                    for k in range(16):
                        p.op("tensor", lambda e, k=k, c=c, tb=tb, pu=pu: e.matmul(pu[:, :], lhsT=wu_s[:, k, c * 128:(c + 1) * 128], rhs=x[:, k, tb * 512:(tb + 1) * 512], start=(k == 0), stop=(k == 15)), reads=[wu_s, x], writes=[pu], inc=(k == 15))
                    s_ = sgb[si % 2]; si += 1
                    p.op("scalar", lambda e, pg=pg, s_=s_: e.activation(out=s_[:], in_=pg[:, :], func=AF.Silu), reads=[pg], writes=[s_])
                    p.op("vector", lambda e, pu=pu, s_=s_, c=c, tb=tb, h=h: e.tensor_tensor(out=h[:, c, tb * 512:(tb + 1) * 512], in0=pu[:, :], in1=s_[:], op=ALU.mult), reads=[pu, s_, h], writes=[h])
            for t in range(NTT):
                for db in range(4):
                    py = YB.next()
                    for c in range(gc):
                        p.op("tensor", lambda e, c=c, t=t, db=db, py=py: e.matmul(py[:, :], lhsT=h[:, c, t * 128:(t + 1) * 128], rhs=wd_s[:, c, db * 512:(db + 1) * 512], start=(c == 0), stop=(c == gc - 1)), reads=[h, wd_s], writes=[py], inc=(c == gc - 1))
                    if g == 0:
                        p.op("scalar", lambda e, t=t, db=db, py=py: e.copy(out=yacc[:, t, db * 512:(db + 1) * 512], in_=py[:, :]), reads=[py, yacc], writes=[yacc])
                    else:
                        p.op("vector", lambda e, t=t, db=db, py=py: e.tensor_tensor(out=yacc[:, t, db * 512:(db + 1) * 512], in0=py[:, :], in1=yacc[:, t, db * 512:(db + 1) * 512], op=ALU.add), reads=[py, yacc], writes=[yacc])
        for t in range(NTT):
            r0 = ps_ * TP + t * 128
            yo = yob[t % 2]
            p.op("gpsimd", lambda e, t=t, yo=yo: e.tensor_copy(out=yo[:], in_=yacc[:, t, :]), reads=[yacc], writes=[yo])
            p.dma("sync", y_out[r0:r0 + 128, :], yo[:], reads=[yo], sembuf=yo)
    p.finish()
    print("ffn instr", p.ninstr, "sems", len(p.sems))
    return p.emit()


import numpy as np
OFF = np.cumsum([0, 768, 768, 768, 768, 6, 6, 6, 6, 768, 256, 256, 256, 256, 512, 512, 16, 16])
NAMES = ["dq", "dk", "dv", "dgate", "a_f", "a_b", "b_f", "b_b", "aq", "ak", "av", "gq", "gkk", "gv", "ggate", "lr_f", "lr_b"]
COL = {n: int(OFF[i]) for i, n in enumerate(NAMES)}


def kmajor(w):
    M = w.shape[1]
    return np.ascontiguousarray(w.reshape(16, 128, M).transpose(1, 0, 2).reshape(128, 16 * M))


def xblocks(xT):
    D, S = xT.shape
    NB = S // 256
    xp = np.zeros((D, S + 4), xT.dtype)
    xp[:, 2:S + 2] = xT
    out = np.empty((NB, 128, 16 * 260), xT.dtype)
    for b in range(NB):
        blk = xp[:, 256 * b:256 * b + 260]
        out[b] = blk.reshape(16, 128, 260).transpose(1, 0, 2).reshape(128, 16 * 260)
    return out


def unit_ids(p):
    return dict(hA=p, hB=4 + p // 2, half=p % 2, gh=p, kv=p // 2, qhalf=p % 2)


def mixer_weights(w_in, dn_conv, a_log, dt_bias, qn_g, kn_g, gla_up, gla_up_b, p):
    u = unit_ids(p)
    hA, hB, half, gh = u["hA"], u["hB"], u["half"], u["gh"]
    c = COL
    def cols(name, a, n):
        return w_in[:, c[name] + a:c[name] + a + n]
    fm = np.concatenate([cols("dq", hA * 128, 128), cols("dk", hA * 128, 128), cols("dv", hA * 128, 128),
                         cols("dq", hB * 128, 128), cols("dk", hB * 128, 128), cols("dv", hB * 128 + half * 64, 64),
                         cols("gq", gh * 64, 64), cols("gkk", gh * 64, 64), cols("lr_f", 0, 16), cols("lr_b", 0, 16)], axis=1)
    tm = np.concatenate([cols("gv", gh * 128, 128),
                         cols("a_f", hA, 1), cols("a_f", hB, 1), cols("b_f", hA, 1), cols("b_f", hB, 1),
                         cols("a_b", hA, 1), cols("a_b", hB, 1), cols("b_b", hA, 1), cols("b_b", hB, 1),
                         cols("dgate", hA * 128, 128), cols("dgate", hB * 128 + half * 64, 64), cols("ggate", gh * 128, 128)], axis=1)
    prm = np.zeros((128, 64), np.float32)
    chans = [(0 + hA * 128, 128), (768 + hA * 128, 128), (1536 + hA * 128, 128),
             (0 + hB * 128, 128), (768 + hB * 128, 128), (1536 + hB * 128 + half * 64, 64)]
    for ti, (c0, M) in enumerate(chans):
        prm[0:M, 5 * ti:5 * ti + 5] = dn_conv[:, c0:c0 + M].T
    prm[:, 30:34] = np.array([a_log[0, hA], a_log[0, hB], a_log[1, hA], a_log[1, hB]])[None, :]
    prm[:, 34:38] = np.array([dt_bias[0, hA], dt_bias[0, hB], dt_bias[1, hA], dt_bias[1, hB]])[None, :]
    prm[:, 38] = qn_g
    prm[:, 39] = kn_g
    prm[0:64, 40] = gla_up_b[0, gh * 64:(gh + 1) * 64]
    prm[0:64, 41] = gla_up_b[1, gh * 64:(gh + 1) * 64]
    up = np.concatenate([gla_up[0][:, gh * 64:(gh + 1) * 64], gla_up[1][:, gh * 64:(gh + 1) * 64]], axis=1)
    return dict(wfm=kmajor(np.ascontiguousarray(fm)), wtm=kmajor(np.ascontiguousarray(tm)), prm=prm, glaup=np.ascontiguousarray(up.astype(np.float32)))


def rope_tables(S):
    t = np.arange(S)
    row = (t // 64).astype(np.float32)
    col = (t % 64).astype(np.float32)
    inv = (10000.0 ** (-np.arange(0, 64, 2, dtype=np.float32) / 64)).astype(np.float32)
    cos = np.empty((128, S), np.float32); sin = np.empty((128, S), np.float32)
    for d in range(128):
        pos = row if d < 64 else col
        ang = (pos * inv[d % 32]).astype(np.float32)
        cos[d] = np.cos(ang); sin[d] = np.sin(ang)
    return cos, sin


def att_weights(w_in, p):
    u = unit_ids(p)
    kv = u["kv"]
    c = COL
    fa = np.concatenate([w_in[:, c["aq"] + (3 * kv + g) * 128:c["aq"] + (3 * kv + g + 1) * 128] for g in range(3)]
                        + [w_in[:, c["ak"] + kv * 128:c["ak"] + (kv + 1) * 128]], axis=1)
    ta = w_in[:, c["av"] + kv * 128:c["av"] + (kv + 1) * 128]
    return dict(wfa=kmajor(np.ascontiguousarray(fa)), wta=kmajor(np.ascontiguousarray(ta)))


import ml_dtypes


def build_dn(S, xdt):
    p = PB(); nc = p.nc
    NB = S // 256
    ext = lambda n, s, d: nc.dram_tensor(n, s, d, kind="ExternalInput").ap()
    out = lambda n, s: nc.dram_tensor(n, s, F32, kind="ExternalOutput").ap()
    xb = ext("xb", [NB, 128, 16 * 260], xdt)
    wfm = ext("wfm", [128, 16 * FM1], F32); wtm = ext("wtm", [128, 16 * TM1], F32)
    prm = ext("prm", [128, 64], F32); glaup = ext("glaup", [16, 128], F32); cst = ext("cst", [128, len(CN) * 128], F32)
    oA = out("oA", [S, 128]); oB = out("oB", [S, 64]); ogl = out("ogl", [S, 128]); gts = out("gts", [S, 320])
    ofw = nc.dram_tensor("ofw", [S, 320], F32).ap()
    phase_dngla(p, S, xdt, xb, wfm, wtm, prm, glaup, cst, oA, oB, ogl, gts, ofw, chdt=F32)
    p.finish()
    return p.emit()


def build_mix(S, xdt):
    p = PB(); nc = p.nc
    NB = S // 256; NQB = NB // 2; SQ = S // 2
    ext = lambda n, s, d: nc.dram_tensor(n, s, d, kind="ExternalInput").ap()
    out = lambda n, s: nc.dram_tensor(n, s, F32, kind="ExternalOutput").ap()
    xb = ext("xb", [NB, 128, 16 * 260], xdt); xq = ext("xq", [NQB, 128, 16 * 260], xdt)
    wfm = ext("wfm", [128, 16 * FM1], F32); wtm = ext("wtm", [128, 16 * TM1], F32)
    prm = ext("prm", [128, 64], F32); glaup = ext("glaup", [16, 128], F32); cst = ext("cst", [128, len(CN) * 128], F32)
    wfa = ext("wfa", [128, 16 * 512], F32); wta = ext("wta", [128, 16 * 128], F32)
    cosk = ext("cosk", [128, S], F32); sink = ext("sink", [128, S], F32)
    cosq = ext("cosq", [128, SQ], F32); sinq = ext("sinq", [128, SQ], F32)
    oA = out("oA", [S, 128]); oB = out("oB", [S, 64]); ogl = out("ogl", [S, 128]); gts = out("gts", [S, 320]); oat = out("oat", [SQ, 384])
    ofw = nc.dram_tensor("ofw", [S, 320], F32).ap()
    banks = [p.ps(f"bank{i}", [128, 512], F32) for i in range(8)]
    p.push()
    phase_dngla(p, S, xdt, xb, wfm, wtm, prm, glaup, cst, oA, oB, ogl, gts, ofw, chdt=F32, banks=banks)
    p.pop()
    p.push()
    phase_att(p, S, xdt, banks, xb, xq, wfa, wta, prm, cst, cosk, sink, cosq, sinq, oat)
    p.pop()
    p.finish()
    return p.emit()


_NC = {}


def _get(key, fn):
    if key not in _NC:
        _NC[key] = fn()
    return _NC[key]


import time as _time
from sys import stderr as _stderr
_T0 = [_time.time()]


def _log(msg):
    if os.environ.get("K_LOG"):
        print(f"[k {_time.time() - _T0[0]:7.1f}s] {msg}", file=_stderr, flush=True)


def _run(nc, maps):
    t = _time.time()
    r = run_bass_kernel_spmd(nc, maps, core_ids=list(range(len(maps)))).results
    _log(f"launch done in {_time.time() - t:.1f}s")
    return r


def kernel(x, w_in, dn_conv, dn_a_log, dn_dt_bias, dn_norm_g, att_qn_g, att_kn_g, gla_up, gla_up_b, gla_norm_g, w_out,
           ln1_g, ln1_b, ln2_g, ln2_b, ffn_w_gate, ffn_w_up, ffn_w_down, router_w, exp_w_gate, exp_w_up, exp_w_down):
    f32 = np.float32
    x = np.asarray(x, f32)
    B, S, D = x.shape
    L = w_in.shape[0]
    T = B * S; NCORE = 8; TC = T // NCORE; TT = TC // 128
    NB = S // 256; SQ = S // 2
    DFF = ffn_w_gate.shape[2]; FD = DFF // NCORE
    TP = 1024 if T % 1024 == 0 and TC >= 1024 else 512
    NP = T // TP
    cst = make_consts(); cos, sin = rope_tables(S); ident = np.eye(128, dtype=f32)
    xcur = np.ascontiguousarray(x.reshape(T, D))
    xT_b = [np.ascontiguousarray(x[b].T) for b in range(B)]
    bdt = {True: F32, False: BF16}
    _T0[0] = _time.time()
    for l in range(L):
        _log(f'layer {l}')
        xdt = F32 if l == 0 else BF16
        xbs = [xblocks(xT_b[b]) for b in range(B)]
        mws = [mixer_weights(np.asarray(w_in[l]), np.asarray(dn_conv[l]), np.asarray(dn_a_log[l]), np.asarray(dn_dt_bias[l]),
                             np.asarray(att_qn_g[l]), np.asarray(att_kn_g[l]), np.asarray(gla_up[l]), np.asarray(gla_up_b[l]), p_) for p_ in range(4)]
        _log('DeltaNet + GLA launch')
        _log('attention launch')
        nc_mx = _get(("mix", S, l == 0), lambda: build_mix(S, xdt))
        maps = []
        for c in range(NCORE):
            b, p_ = divmod(c, 4); qh = p_ % 2
            maps.append(dict(xb=xbs[b], xq=np.ascontiguousarray(xbs[b][qh * NB // 2:(qh + 1) * NB // 2]), cst=cst, **mws[p_],
                             cosk=cos, sink=sin, cosq=np.ascontiguousarray(cos[:, qh * SQ:(qh + 1) * SQ]),
                             sinq=np.ascontiguousarray(sin[:, qh * SQ:(qh + 1) * SQ]), **att_weights(np.asarray(w_in[l]), p_)))
        r_at = _run(nc_mx, maps)
        r_dn = r_at
        del xbs, maps
        o = np.empty((T, 2048), f32); gt = np.empty((T, 1280), f32)
        for c in range(NCORE):
            b, p_ = divmod(c, 4); hB = 4 + p_ // 2; half = p_ % 2; kv = p_ // 2; qh = p_ % 2
            rows = slice(b * S, (b + 1) * S)
            rd = r_dn[c]
            o[rows, p_ * 128:(p_ + 1) * 128] = rd["oA"]
            o[rows, hB * 128 + half * 64:hB * 128 + half * 64 + 64] = rd["oB"]
            o[rows, 1536 + p_ * 128:1536 + (p_ + 1) * 128] = rd["ogl"]
            o[b * S + qh * SQ:b * S + (qh + 1) * SQ, 768 + 3 * kv * 128:768 + 3 * kv * 128 + 384] = r_at[c]["oat"]
            g_ = rd["gts"]
            gt[rows, p_ * 128:(p_ + 1) * 128] = g_[:, 0:128]
            gt[rows, hB * 128 + half * 64:hB * 128 + half * 64 + 64] = g_[:, 128:192]
            gt[rows, 768 + p_ * 128:768 + (p_ + 1) * 128] = g_[:, 192:320]
        del r_dn, r_at
        _log('post launch (token para')
        moe = (l % 2 == 1); j = l // 2
        nc_po = _get(("post", TT, moe), lambda: build_post(TT, moe))
        wout_k = kmajor(np.asarray(w_out[l], f32))
        nrm = np.tile(np.concatenate([np.asarray(dn_norm_g[l], f32), np.asarray(gla_norm_g[l], f32)])[None], (128, 1))
        ln1 = np.tile(np.concatenate([np.asarray(ln1_g[l], f32), np.asarray(ln1_b[l], f32)])[None], (128, 1))
        maps = []
        for c in range(NCORE):
            sl = slice(c * TC, (c + 1) * TC)
            m = dict(o=o[sl], gt=gt[sl], x=xcur[sl], wout=wout_k, nrm=nrm, ln=ln1, ident=ident)
            if moe:
                m["rw"] = kmajor(np.asarray(router_w[j], f32))
            maps.append(m)
        r_po = _run(nc_po, maps)
        del o, gt, maps
        x1 = [r_po[c]["x1"] for c in range(NCORE)]
        gates = [r_po[c]["gates"] for c in range(NCORE)] if moe else None
        x1T = np.stack([np.asarray(r_po[c]["x1T"]).reshape(TT, 128, 16, 128) for c in range(NCORE)]).reshape(NP, TP // 128, 128, 16, 128)
        xT_all = np.ascontiguousarray(x1T.transpose(0, 2, 3, 1, 4).reshape(NP, 128, 16 * TP))
        del r_po, x1T
        _log('FFN launch (expert / ff')
        F_ = DFF if moe else FD
        gc = 2 if moe else 1
        nc_ff = _get(("ffn", NP, F_, gc, TP), lambda: build_ffn(NP, F_, gc, TP))
        maps = []
        for e in range(NCORE):
            if moe:
                wg_ = np.asarray(exp_w_gate[j][e], f32); wu_ = np.asarray(exp_w_up[j][e], f32); wd_ = np.asarray(exp_w_down[j][e], f32)
            else:
                wg_ = np.asarray(ffn_w_gate[j][:, e * FD:(e + 1) * FD], f32); wu_ = np.asarray(ffn_w_up[j][:, e * FD:(e + 1) * FD], f32)
                wd_ = np.asarray(ffn_w_down[j][e * FD:(e + 1) * FD, :], f32)
            wdl = np.ascontiguousarray(wd_.reshape(F_ // 128, 128, 2048).transpose(1, 0, 2).reshape(128, (F_ // 128) * 2048))
            maps.append(dict(xT=xT_all, wg=kmajor(np.ascontiguousarray(wg_)), wu=kmajor(np.ascontiguousarray(wu_)), wd=wdl))
        r_ff = _run(nc_ff, maps)
        del maps, xT_all
        _log('combine launch (token p')
        nc_cb = _get(("comb", TT, moe), lambda: build_combine(TT, moe))
        ln2 = np.tile(np.concatenate([np.asarray(ln2_g[l], f32), np.asarray(ln2_b[l], f32)])[None], (128, 1))
        maps = []
        for c in range(NCORE):
            sl = slice(c * TC, (c + 1) * TC)
            m = dict(x1=x1[c], y=np.stack([r_ff[e]["y"][sl] for e in range(NCORE)]), ln=ln2, ident=ident)
            if moe:
                m["gates"] = gates[c]
            maps.append(m)
        del r_ff
        r_cb = _run(nc_cb, maps)
        del maps
        xcur = np.concatenate([r_cb[c]["x2"] for c in range(NCORE)], axis=0)
        xT_b = []
        for b in range(B):
            cols = []
            for c in range(4 * b, 4 * b + 4):
                a = np.asarray(r_cb[c]["x2T"]).reshape(TT, 128, 16, 128).transpose(2, 1, 0, 3).reshape(2048, TC)
                cols.append(a)
            xT_b.append(np.ascontiguousarray(np.concatenate(cols, axis=1)))
        del r_cb
    return np.ascontiguousarray(xcur.reshape(B, S, D).astype(f32))
```
